# Optimizing a Trainium2 kernel written in Bass

```python
import jax, jax.numpy as jnp
from jax import lax
import numpy as np

D_MODEL = 2048
BATCH = 16
SEQ = 2048
DEPTH = 1

ATT_WIDTH = D_MODEL // 2
ATT_HEAD_DIM = 64
ATT_HEADS = ATT_WIDTH // ATT_HEAD_DIM
ATT_KV_HEADS = 4
ATT_KV_WIDTH = ATT_KV_HEADS * ATT_HEAD_DIM
WINDOW = 128
ATT_BLOCK = 128
ML_WIDTH = D_MODEL // 2
ML_HEADS = 4
ML_HEAD_DIM = ML_WIDTH // ML_HEADS
ML_CHUNK = 64
CONV_WIDTH = 4
F_BIAS_LO = 3.0
F_BIAS_HI = 6.0
N_BRANCHES = 2
PEER_HEADS = 8
N_KEYS = 128
N_EXPERTS = N_KEYS * N_KEYS
PEER_KEY_DIM = 128
PEER_TOPK_HALF = 16
PEER_TOPK = 16
PEER_TOKEN_BLOCK = 128

EPS = 1e-6
NEG_INF = -1e30

IN_SIZES = (ATT_WIDTH, ATT_KV_WIDTH, ATT_KV_WIDTH,
            ML_WIDTH, ML_WIDTH, ML_WIDTH, ML_WIDTH, ML_HEADS, ML_HEADS,
            N_BRANCHES * D_MODEL)
IN_WIDTH = sum(IN_SIZES)
F_OFFSET = ATT_WIDTH + 2 * ATT_KV_WIDTH + 4 * ML_WIDTH + ML_HEADS

kernel_name = "hybrid_swa_mlstm_peer_block"


def rms_norm(x, g):
    xf = x.astype(jnp.float32)
    y = xf * lax.rsqrt(jnp.mean(xf * xf, axis=-1, keepdims=True) + EPS)
    return (y * g.astype(jnp.float32)).astype(x.dtype)


def causal_depthwise_conv(x, w, b):
    C = x.shape[-1]
    y = lax.conv_general_dilated(x, w[:, None, :].astype(x.dtype), window_strides=(1,),
                                 padding=[(CONV_WIDTH - 1, 0)],
                                 dimension_numbers=('NWC', 'WIO', 'NWC'),
                                 feature_group_count=C)
    return y + b


def sliding_window_attention(q, k, v, sinks):
    B, S, _, dh = q.shape
    nb = S // ATT_BLOCK
    G = ATT_HEADS // ATT_KV_HEADS
    qb = q.reshape(B, nb, ATT_BLOCK, ATT_KV_HEADS, G, dh).swapaxes(0, 1)

    def band(a):
        ap = jnp.pad(a, ((0, 0), (ATT_BLOCK, 0), (0, 0), (0, 0)))
        ap = ap.reshape(B, nb + 1, ATT_BLOCK, ATT_KV_HEADS, dh)
        return jnp.concatenate([ap[:, :-1], ap[:, 1:]], axis=2).swapaxes(0, 1)

    kb, vb = band(k), band(v)
    q_pos = jnp.arange(ATT_BLOCK)[:, None]
    k_pos = jnp.arange(2 * ATT_BLOCK)[None, :] - ATT_BLOCK
    diff = q_pos - k_pos
    blk_start = jnp.arange(nb)[:, None, None] * ATT_BLOCK
    valid = (diff >= 0) & (diff < WINDOW) & (blk_start + k_pos >= 0)
    sink = sinks.astype(jnp.float32).reshape(ATT_KV_HEADS, G)[None, :, :, None, None]
    scale = dh ** -0.5

    def one_block(args):
        qi, ki, vi, mi = args
        s = jnp.einsum('bqhgd,bkhd->bhgqk', qi.astype(jnp.float32), ki.astype(jnp.float32)) * scale
        s = jnp.where(mi[None, None, None], s, NEG_INF)
        m = jnp.maximum(s.max(axis=-1, keepdims=True), sink)
        p = jnp.exp(s - m)
        p = p / (p.sum(axis=-1, keepdims=True) + jnp.exp(sink - m))
        return jnp.einsum('bhgqk,bkhd->bqhgd', p, vi.astype(jnp.float32))

    o = lax.map(one_block, (qb, kb, vb, valid))
    return o.swapaxes(0, 1).reshape(B, S, ATT_WIDTH).astype(q.dtype)


def mlstm_chunkwise(q, k, v, i_pre, f_pre):
    B, S, H, d = q.shape
    L = ML_CHUNK
    nc = S // L
    q = q.astype(jnp.float32)
    k = k.astype(jnp.float32) * (d ** -0.5)
    v = v.astype(jnp.float32)
    i_pre = i_pre.astype(jnp.float32)
    log_f = jax.nn.log_sigmoid(f_pre.astype(jnp.float32))

    def to_chunks(a):
        a = a.reshape(B, nc, L, H, *a.shape[3:])
        return jnp.moveaxis(a, (1, 3), (0, 2))

    tri = jnp.tril(jnp.ones((L, L), dtype=bool))

    def step(carry, xs):
        C, n, m = carry
        qc, kc, vc, ic, lfc = xs
        b = jnp.cumsum(lfc, axis=-1)
        logD = jnp.where(tri, b[..., :, None] - b[..., None, :] + ic[..., None, :], NEG_INF)
        m_inter = b + m[..., None]
        m_t = jnp.maximum(logD.max(axis=-1), m_inter)
        Dm = jnp.exp(logD - m_t[..., None])
        Sc = jnp.einsum('bhtd,bhsd->bhts', qc, kc) * Dm
        w_inter = jnp.exp(m_inter - m_t)
        num = (jnp.einsum('bhts,bhsd->bhtd', Sc, vc)
               + w_inter[..., None] * jnp.einsum('bhtk,bhvk->bhtv', qc, C))
        den = Sc.sum(axis=-1) + w_inter * jnp.einsum('bhtk,bhk->bht', qc, n)
        h = num / jnp.maximum(jnp.abs(den), jnp.exp(-m_t))[..., None]
        bL = b[..., -1]
        log_w = bL[..., None] - b + ic
        m_new = jnp.maximum(bL + m, log_w.max(axis=-1))
        w = jnp.exp(log_w - m_new[..., None])
        decay = jnp.exp(bL + m - m_new)
        C_new = decay[..., None, None] * C + jnp.einsum('bhs,bhsv,bhsk->bhvk', w, vc, kc)
        n_new = decay[..., None] * n + jnp.einsum('bhs,bhsk->bhk', w, kc)
        return (C_new, n_new, m_new), h

    init = (jnp.zeros((B, H, d, d), jnp.float32), jnp.zeros((B, H, d), jnp.float32),
            jnp.full((B, H), NEG_INF, jnp.float32))
    _, hs = lax.scan(step, init, (to_chunks(q), to_chunks(k), to_chunks(v),
                                  to_chunks(i_pre), to_chunks(log_f)))
    return jnp.moveaxis(hs, (0, 2), (1, 3)).reshape(B, S, H, d)


def peer(xn, w_peer_q, peer_keys, peer_u, peer_v):
    B, S, D = xn.shape
    T = B * S
    xt = xn.reshape(T, D)
    qp = (xt @ w_peer_q).reshape(T, PEER_HEADS, 2, PEER_KEY_DIM)
    scores = jnp.einsum('thcd,hcnd->thcn', qp.astype(jnp.float32), peer_keys.astype(jnp.float32))
    s_top, i_top = lax.top_k(scores, PEER_TOPK_HALF)
    cand = (s_top[:, :, 0, :, None] + s_top[:, :, 1, None, :]).reshape(T, PEER_HEADS, -1)
    cand_idx = (i_top[:, :, 0, :, None] * N_KEYS + i_top[:, :, 1, None, :]).reshape(T, PEER_HEADS, -1)
    top_s, pos = lax.top_k(cand, PEER_TOPK)
    idx = jnp.take_along_axis(cand_idx, pos, axis=-1)
    gates = jax.nn.softmax(top_s, axis=-1).astype(xn.dtype)
    nblk = T // PEER_TOKEN_BLOCK

    def block(args):
        xb, ib, gb = args
        u = peer_u[ib]
        act = jax.nn.gelu(jnp.einsum('td,thkd->thk', xb, u), approximate=False)
        return jnp.einsum('thk,thkd->td', gb * act, peer_v[ib])

    out = lax.map(block, (xt.reshape(nblk, PEER_TOKEN_BLOCK, D),
                          idx.reshape(nblk, PEER_TOKEN_BLOCK, PEER_HEADS, PEER_TOPK),
                          gates.reshape(nblk, PEER_TOKEN_BLOCK, PEER_HEADS, PEER_TOPK)))
    return out.reshape(B, S, D)


def hybrid_layer(x, norm_mix_g, w_in, b_in, conv_w, conv_b, q_norm_g, k_norm_g, sinks,
                 ml_norm_g, w_proj_att, w_proj_ml, w_out, norm_ffn_g, w_peer_q,
                 peer_keys, peer_u, peer_v):
    B, S, _ = x.shape
    xn = rms_norm(x, norm_mix_g)
    proj = xn @ w_in + b_in
    split_idx = [int(c) for c in np.cumsum(IN_SIZES)[:-1]]
    a_q, a_k, a_v, m_q, m_k, m_v, m_o, m_i, m_f, g_pre = jnp.split(proj, split_idx, axis=-1)
    a_q = rms_norm(a_q.reshape(B, S, ATT_HEADS, ATT_HEAD_DIM), q_norm_g)
    a_k = rms_norm(a_k.reshape(B, S, ATT_KV_HEADS, ATT_HEAD_DIM), k_norm_g)
    a_v = a_v.reshape(B, S, ATT_KV_HEADS, ATT_HEAD_DIM)
    att = sliding_window_attention(a_q, a_k, a_v, sinks)
    qk = jax.nn.silu(causal_depthwise_conv(jnp.concatenate([m_q, m_k], axis=-1), conv_w, conv_b))
    m_q, m_k = jnp.split(qk, 2, axis=-1)
    h = mlstm_chunkwise(m_q.reshape(B, S, ML_HEADS, ML_HEAD_DIM),
                        m_k.reshape(B, S, ML_HEADS, ML_HEAD_DIM),
                        m_v.reshape(B, S, ML_HEADS, ML_HEAD_DIM), m_i, m_f)
    h = rms_norm(h, ml_norm_g.reshape(ML_HEADS, ML_HEAD_DIM)).reshape(B, S, ML_WIDTH).astype(x.dtype)
    ml = jax.nn.sigmoid(m_o) * h
    g = jax.nn.sigmoid(g_pre).reshape(B, S, N_BRANCHES, D_MODEL)
    merged = g[:, :, 0] * (att @ w_proj_att) + g[:, :, 1] * (ml @ w_proj_ml)
    x = x + merged @ w_out
    x = x + peer(rms_norm(x, norm_ffn_g), w_peer_q, peer_keys, peer_u, peer_v)
    return x


def setup_inputs(seed: int = 0) -> dict:
    key = jax.random.key(seed)
    ks = jax.random.split(key, 20)
    f32 = jnp.float32

    def nrm(k, shape, scale):
        return jax.random.normal(k, shape, f32) * scale

    Dp = DEPTH
    b_in = nrm(ks[3], (Dp, IN_WIDTH), 0.02)
    b_in = b_in.at[:, F_OFFSET:F_OFFSET + ML_HEADS].add(jnp.linspace(F_BIAS_LO, F_BIAS_HI, ML_HEADS))
    return {
        "x": nrm(ks[0], (BATCH, SEQ, D_MODEL), 1.0),
        "norm_mix_g": 1.0 + nrm(ks[1], (Dp, D_MODEL), 0.02),
        "w_in": nrm(ks[2], (Dp, D_MODEL, IN_WIDTH), D_MODEL ** -0.5),
        "b_in": b_in,
        "conv_w": nrm(ks[4], (Dp, CONV_WIDTH, 2 * ML_WIDTH), CONV_WIDTH ** -0.5),
        "conv_b": nrm(ks[5], (Dp, 2 * ML_WIDTH), 0.02),
        "q_norm_g": 1.0 + nrm(ks[6], (Dp, ATT_HEAD_DIM), 0.02),
        "k_norm_g": 1.0 + nrm(ks[7], (Dp, ATT_HEAD_DIM), 0.02),
        "sinks": nrm(ks[8], (Dp, ATT_HEADS), 0.5),
        "ml_norm_g": 1.0 + nrm(ks[9], (Dp, ML_WIDTH), 0.02),
        "w_proj_att": nrm(ks[10], (Dp, ATT_WIDTH, D_MODEL), ATT_WIDTH ** -0.5),
        "w_proj_ml": nrm(ks[11], (Dp, ML_WIDTH, D_MODEL), ML_WIDTH ** -0.5),
        "w_out": nrm(ks[12], (Dp, D_MODEL, D_MODEL), D_MODEL ** -0.5),
        "norm_ffn_g": 1.0 + nrm(ks[13], (Dp, D_MODEL), 0.02),
        "w_peer_q": nrm(ks[14], (Dp, D_MODEL, PEER_HEADS * 2 * PEER_KEY_DIM), D_MODEL ** -0.5),
        "peer_keys": nrm(ks[15], (Dp, PEER_HEADS, 2, N_KEYS, PEER_KEY_DIM), PEER_KEY_DIM ** -0.5),
        "peer_u": nrm(ks[16], (Dp, N_EXPERTS, D_MODEL), D_MODEL ** -0.5),
        "peer_v": nrm(ks[17], (Dp, N_EXPERTS, D_MODEL), PEER_HEADS ** -0.5),
    }


def reference(x, norm_mix_g, w_in, b_in, conv_w, conv_b, q_norm_g, k_norm_g, sinks,
              ml_norm_g, w_proj_att, w_proj_ml, w_out, norm_ffn_g, w_peer_q,
              peer_keys, peer_u, peer_v):
    for l in range(DEPTH):
        x = hybrid_layer(x, norm_mix_g[l], w_in[l], b_in[l], conv_w[l], conv_b[l],
                         q_norm_g[l], k_norm_g[l], sinks[l], ml_norm_g[l],
                         w_proj_att[l], w_proj_ml[l], w_out[l], norm_ffn_g[l],
                         w_peer_q[l], peer_keys[l], peer_u[l], peer_v[l])
    return x
```

```python
from contextlib import ExitStack
import numpy as np
import concourse.bass as bass
import concourse.mybir as mybir
from concourse.bass_utils import run_bass_kernel_spmd

F32 = mybir.dt.float32
BF16 = mybir.dt.bfloat16
U32 = mybir.dt.uint32
I32 = mybir.dt.int32
ACT = mybir.ActivationFunctionType
ALU = mybir.AluOpType
AX = mybir.AxisListType

D = 2048
KD = 16
INW = 9736
EPS = 1e-6
NEXP = 16384
P0_CHUNKS = 128


class Sched:
    NDS = 24

    def __init__(self, nc, es):
        self.nc = nc
        self.eng = {"pe": nc.tensor, "act": nc.scalar, "dve": nc.vector, "pool": nc.gpsimd, "sp": nc.sync}
        self.sem = {}
        for k in self.eng:
            self.sem[k] = es.enter_context(nc.semaphore("s_" + k))
        for i in range(self.NDS):
            self.sem["d%d" % i] = es.enter_context(nc.semaphore("sd%d" % i))
        self.cnt = {k: 0 for k in self.sem}
        self.seen = {k: {} for k in self.eng}
        self.dnext = 0
        self.lastw = {}
        self.readers = {}
        self.nins = 0
        self.stores = {}

    def barrier(self, e):
        for k, v in self.stores.items():
            self._wait(e, k, v)

    def full_barrier(self):
        for e in self.eng:
            for k in self.sem:
                v = self.cnt[k] * (16 if k[1:].isdigit() else 1)
                if v > 0:
                    self._wait(e, k, v)

    def _wait(self, e, key, val):
        if e == "pe" and key == "pe":
            return
        if self.seen[e].get(key, 0) >= val:
            return
        self.eng[e].wait_ge(self.sem[key], val)
        self.seen[e][key] = val

    def _deps(self, e, reads, writes):
        need = {}
        for r in reads:
            t = self.lastw.get(r)
            if t is not None:
                need[t[0]] = max(need.get(t[0], 0), t[1])
        for w in writes:
            t = self.lastw.get(w)
            if t is not None:
                need[t[0]] = max(need.get(t[0], 0), t[1])
            for k, v in self.readers.get(w, {}).items():
                need[k] = max(need.get(k, 0), v)
        for k, v in need.items():
            self._wait(e, k, v)

    def _record(self, tok, reads, writes):
        for r in reads:
            d = self.readers.setdefault(r, {})
            d[tok[0]] = max(d.get(tok[0], 0), tok[1])
        for w in writes:
            self.lastw[w] = tok
            self.readers[w] = {}

    def op(self, e, fn, reads=(), writes=(), inc=True):
        self._deps(e, reads, writes)
        ins = fn(self.eng[e])
        if inc:
            self.cnt[e] += 1
            ins.then_inc(self.sem[e], 1)
            tok = (e, self.cnt[e])
        else:
            assert e == "pe"
            tok = (e, self.cnt[e] + 1)
        self._record(tok, reads, writes)
        self.nins += 1

    def dma(self, q, out, in_, reads=(), writes=(), **kw):
        slot = self.dnext
        self.dnext = (slot + 1) % self.NDS
        key = "d%d" % slot
        if self.cnt[key] > 0:
            self._wait(q, key, self.cnt[key] * 16)
        self._deps(q, reads, writes)
        ins = self.eng[q].dma_start(out=out, in_=in_, **kw)
        self.cnt[key] += 1
        ins.then_inc(self.sem[key], 16)
        self._record((key, self.cnt[key] * 16), reads, writes)
        self.stores[key] = self.cnt[key] * 16
        self.nins += 1

    def wait_all(self, e):
        for r, t in list(self.lastw.items()):
            self._wait(e, t[0], t[1])


def build_program(NSEQ, SEQ, phases=(0, 1, 2, 3, 4, 5), debug=False):
    T = NSEQ * SEQ
    NT = T // 128
    TPS = SEQ // 128
    GT = min(512, SEQ)
    NTG = GT // 128
    NG = T // GT
    nc = bass.Bass("TRN2", target_bir_lowering=False)
    es = ExitStack()

    def din(name, shape):
        return nc.dram_tensor(name, list(shape), F32, kind="ExternalInput").ap()

    dbg_kind = "ExternalOutput" if debug else "Internal"

    def dscr(name, shape, dt):
        return nc.dram_tensor(name, list(shape), dt, kind=dbg_kind).ap()

    x = din("x", [T, D])
    norm_mix_g = din("norm_mix_g", [1, D])
    w_in = din("w_in", [D, INW])
    b_in = din("b_in", [1, INW])
    conv_w = din("conv_w", [4, 2048])
    conv_b = din("conv_b", [1, 2048])
    q_norm_g = din("q_norm_g", [1, 64])
    k_norm_g = din("k_norm_g", [1, 64])
    sinks = din("sinks", [1, 16])
    ml_norm_g = din("ml_norm_g", [1, 1024])
    w_proj_att = din("w_proj_att", [1024, D])
    w_proj_ml = din("w_proj_ml", [1024, D])
    w_out = din("w_out", [D, D])
    norm_ffn_g = din("norm_ffn_g", [1, D])
    if 0 in phases or 5 in phases:
        w_peer_q = din("w_peer_q", [D, D])
        peer_keys = din("peer_keys", [16, 128, 128])
        peer_u = din("peer_u", [NEXP, D])
        peer_v = din("peer_v", [NEXP, D])
    y = nc.dram_tensor("y", [T, D], F32, kind="ExternalOutput").ap()

    Ptm = dscr("Ptm", [T, 3584], F32)
    Pqk = dscr("Pqk", [2048, T], F32)
    Pif = dscr("Pif", [8, T], F32)
    Pg = dscr("Pg", [4096, T], F32)
    AttT = dscr("AttT", [1024, T], BF16)
    MlT = dscr("MlT", [1024, T], BF16)
    if 5 in phases and 4 not in phases:
        X1 = din("X1", [T, D])
    else:
        X1 = dscr("X1", [T, D], F32)
    MgT = dscr("MgT", [2048, T], BF16)
    UTs = dscr("UTs", [128, 128, 2048], BF16)
    Vs = dscr("Vs", [128, 128, 2048], BF16)
    WQs = dscr("WQs", [16, 128, KD, 128], BF16)

    S = Sched(nc, es)

    class Rec:
        def __init__(self):
            self.steps = []
            self.grp = None

        def begin(self):
            self.grp = []

        def end(self):
            self.steps.append(("grp", self.grp, None))
            self.grp = None

        def op(self, *a, **k):
            (self.grp if self.grp is not None else self.steps).append(("op", a, k))

        def dma(self, *a, **k):
            (self.grp if self.grp is not None else self.steps).append(("dma", a, k))

    def emit(st):
        kind, a, k = st
        if kind == "grp":
            for x in a:
                emit(x)
        else:
            (S.op if kind == "op" else S.dma)(*a, **k)

    def replay(steps, n):
        while n > 0 and steps:
            emit(steps.pop(0))
            n -= 1
    _uid = [0]

    def uniq(name):
        _uid[0] += 1
        return "%s_u%d" % (name, _uid[0])

    def sb(name, shape, dt):
        return es.enter_context(nc.sbuf_tensor(uniq(name), list(shape), dt))

    ps = [es.enter_context(nc.psum_tensor("ps%d" % i, [128, 512], F32)) for i in range(8)]

    def psb(i):
        return ps[i][:].bitcast(BF16)

    identf = sb("identf", [128, 128], F32)
    identb = sb("identb", [128, 128], BF16)
    S.op("pool", lambda e: e.memset(identf[:], 0.0), writes=["identf"])
    S.op("pool", lambda e: e.affine_select(out=identf[:], in_=identf[:], compare_op=ALU.not_equal, fill=1.0,
                                            base=0, pattern=[[-1, 128]], channel_multiplier=1),
         reads=["identf"], writes=["identf"])
    S.op("dve", lambda e: e.tensor_copy(out=identb[:], in_=identf[:]), reads=["identf"], writes=["identb"])

    def bload(dst, src_row, n, res):
        S.dma("sp", out=dst, in_=src_row.partition_broadcast(dst.shape[0]), writes=[res])


    bank = [0]

    def nbank():
        b = bank[0]
        bank[0] = (b + 1) % 8
        return b

    def p0_record(sb0, nbuf=2):
        Q = Rec()
        ust = [sb0("ust%d" % i, [128, D], F32) for i in range(nbuf)]
        vst = [sb0("vst%d" % i, [128, D], F32) for i in range(nbuf)]
        utb = [sb0("utb%d" % i, [128, KD * 128], BF16) for i in range(nbuf)]
        vbf = [sb0("vbf%d" % i, [128, D], BF16) for i in range(nbuf)]
        for ch in range(P0_CHUNKS):
            sl = ch % nbuf
            Q.dma("sp", out=ust[sl][:], in_=peer_u[ch * 128:(ch + 1) * 128, :], writes=["ust%d" % sl])
            for kq in range(4):
                b = 4 + kq % 2
                Q.begin()
                for j in range(4):
                    k = kq * 4 + j
                    Q.op("pe", lambda e, b=b, j=j, k=k, sl=sl: e.transpose(
                        out=ps[b][:, j * 128:(j + 1) * 128], in_=ust[sl][:, k * 128:(k + 1) * 128], identity=identf[:]),
                        reads=["ust%d" % sl, "identf"], writes=["ps%d" % b], inc=(j == 3))
                Q.op("act" if kq % 2 == 0 else "dve", lambda e, b=b, kq=kq, sl=sl: (
                    e.activation(out=utb[sl][:, kq * 512:(kq + 1) * 512], in_=ps[b][:, :], func=ACT.Copy)
                    if kq % 2 == 0 else e.tensor_copy(out=utb[sl][:, kq * 512:(kq + 1) * 512], in_=ps[b][:, :])),
                    reads=["ps%d" % b], writes=["utb%d_%d" % (sl, kq)])
                Q.end()
            Q.dma("pool", out=UTs[ch], in_=utb[sl][:], reads=["utb%d_%d" % (sl, kq) for kq in range(4)])
            Q.dma("sp", out=vst[sl][:], in_=peer_v[ch * 128:(ch + 1) * 128, :], writes=["vst%d" % sl])
            Q.op("act", lambda e, sl=sl: e.activation(out=vbf[sl][:, 0:1024], in_=vst[sl][:, 0:1024], func=ACT.Copy),
                 reads=["vst%d" % sl], writes=["vbf%da" % sl])
            Q.op("dve", lambda e, sl=sl: e.tensor_copy(out=vbf[sl][:, 1024:2048], in_=vst[sl][:, 1024:2048]),
                 reads=["vst%d" % sl], writes=["vbf%db" % sl])
            Q.dma("pool", out=Vs[ch], in_=vbf[sl][:], reads=["vbf%da" % sl, "vbf%db" % sl])
        for k in range(KD):
            sl = k % nbuf
            Q.dma("sp", out=vst[sl][:], in_=w_peer_q[k * 128:(k + 1) * 128, :], writes=["vst%d" % sl])
            Q.op("act", lambda e, sl=sl: e.activation(out=vbf[sl][:, 0:1024], in_=vst[sl][:, 0:1024], func=ACT.Copy),
                 reads=["vst%d" % sl], writes=["vbf%da" % sl])
            Q.op("dve", lambda e, sl=sl: e.tensor_copy(out=vbf[sl][:, 1024:2048], in_=vst[sl][:, 1024:2048]),
                 reads=["vst%d" % sl], writes=["vbf%db" % sl])
            Q.dma("sp", out=WQs[:, :, k, :].rearrange("b p c -> p b c"), in_=vbf[sl][:].rearrange("p (b c) -> p b c", c=128),
                  reads=["vbf%da" % sl, "vbf%db" % sl])
        return Q.steps

    p0_in_p3 = (0 in phases) and (3 in phases)
    if 0 in phases and not p0_in_p3:
        with ExitStack() as es0:
            def sb0(name, shape, dt):
                return es0.enter_context(nc.sbuf_tensor(uniq(name), list(shape), dt))
            replay(p0_record(sb0), 10 ** 9)
            S.full_barrier()

    if 1 in phases:
        GT1 = min(1024, SEQ)
        NTG1 = GT1 // 128
        NG1 = T // GT1
        HW = min(512, GT1)
        HN = GT1 // HW
        with ExitStack() as es1:
            def sb1(name, shape, dt):
                return es1.enter_context(nc.sbuf_tensor(uniq(name), list(shape), dt))
            gmix = sb1("gmix", [128, D], F32)
            bload(gmix[:], norm_mix_g[0:1, :], D, "gmix")
            bias_tm = sb1("bias_tm", [128, 3584], F32)
            S.dma("sp", out=bias_tm[:, 0:1536], in_=b_in[0:1, 0:1536].partition_broadcast(128), writes=["bias_tm"])
            S.dma("sp", out=bias_tm[:, 1536:3584], in_=b_in[0:1, 3584:5632].partition_broadcast(128), writes=["bias_tm"])
            bcol_qk = sb1("bcol_qk", [128, 16], F32)
            bcol_g = sb1("bcol_g", [128, 32], F32)
            bcol_i = sb1("bcol_i", [4, 1], F32)
            bcol_f = sb1("bcol_f", [4, 1], F32)
            S.dma("sp", out=bcol_qk[:], in_=b_in[0, 1536:3584].rearrange("(b p) -> p b", p=128),
                  writes=["bcol_qk"], allow_slow_non_contiguous=True)
            S.dma("sp", out=bcol_g[:], in_=b_in[0, 5640:9736].rearrange("(b p) -> p b", p=128),
                  writes=["bcol_g"], allow_slow_non_contiguous=True)
            S.dma("sp", out=bcol_i[:], in_=b_in[0, 5632:5636].rearrange("(p b) -> p b", b=1),
                  writes=["bcol_i"], allow_slow_non_contiguous=True)
            S.dma("sp", out=bcol_f[:], in_=b_in[0, 5636:5640].rearrange("(p b) -> p b", b=1),
                  writes=["bcol_f"], allow_slow_non_contiguous=True)

            xt = sb1("xt", [128, D], F32)
            junk = sb1("junk", [128, D], BF16)
            ssq = sb1("ssq", [128, 1], F32)
            rstd = sb1("rstd", [128, 1], F32)
            xnb = sb1("xnb", [128, D], BF16)
            xnT = sb1("xnT", [128, KD, GT1], BF16)
            wsts = [sb1("wst%d" % i, [128, KD, 512], F32) for i in range(2)]
            wbf = [sb1("wbf%d" % i, [128, KD, 512], BF16) for i in range(2)]
            stg = [sb1("stg%d" % i, [128, 512], F32) for i in range(3)]
            stgif = sb1("stgif", [4, HW], F32)
            w_in_v = w_in.rearrange("(k p) n -> p k n", p=128)

            chunks = []
            for c in range(3):
                chunks.append((c * 512, 512, "tm", c * 512))
            for c in range(4):
                chunks.append((1536 + c * 512, 512, "qk", c * 4))
            for c in range(4):
                chunks.append((3584 + c * 512, 512, "tm", 1536 + c * 512))
            chunks.append((5632, 8, "if", 0))
            for c in range(8):
                chunks.append((5640 + c * 512, 512, "g", c * 4))

            bank = [0]
            stgi = [0]

            def nbank():
                b = bank[0]
                bank[0] = (b + 1) % 8
                return b

            def nstg():
                i = stgi[0]
                stgi[0] = (i + 1) % 3
                return i

            for g in range(NG1):
                for ti in range(NTG1):
                    t0 = g * GT1 + ti * 128
                    S.dma("sp", out=xt[:], in_=x[t0:t0 + 128, :], writes=["xt"])
                    S.op("act", lambda e: e.activation(out=junk[:], in_=xt[:], func=ACT.Square, accum_out=ssq[:]),
                         reads=["xt"], writes=["junk", "ssq"])
                    S.op("dve", lambda e: e.tensor_scalar(out=rstd[:], in0=ssq[:], scalar1=1.0 / D, scalar2=EPS,
                                                          op0=ALU.mult, op1=ALU.add), reads=["ssq"], writes=["rstd"])
                    S.op("act", lambda e: e.activation(out=rstd[:], in_=rstd[:], func=ACT.Sqrt),
                         reads=["rstd"], writes=["rstd"])
                    S.op("dve", lambda e: e.reciprocal(out=rstd[:], in_=rstd[:]), reads=["rstd"], writes=["rstd"])
                    S.op("dve", lambda e: e.scalar_tensor_tensor(out=xnb[:], in0=xt[:], scalar=rstd[:, 0:1], in1=gmix[:],
                                                                 op0=ALU.mult, op1=ALU.mult),
                         reads=["xt", "rstd", "gmix"], writes=["xnb"])
                    for kq in range(4):
                        b = nbank()
                        for j in range(4):
                            k = kq * 4 + j
                            S.op("pe", lambda e, k=k, j=j, b=b: e.transpose(out=psb(b)[:, j * 128:(j + 1) * 128],
                                                                            in_=xnb[:, k * 128:(k + 1) * 128],
                                                                            identity=identb[:]),
                                 reads=["xnb", "identb"], writes=["ps%d" % b], inc=(j == 3))
                        S.op("act", lambda e, kq=kq, b=b, ti=ti: e.activation(
                            out=xnT[:, kq * 4:(kq + 1) * 4, ti * 128:(ti + 1) * 128],
                            in_=psb(b)[:, 0:512].rearrange("p (j t) -> p j t", j=4), func=ACT.Copy),
                            reads=["ps%d" % b], writes=["xnT"])
                for ci, (c0, ncol, kind, aux) in enumerate(chunks):
                    wb = wbf[ci % 2]
                    wres = "wbf%d" % (ci % 2)
                    wst = wsts[ci % 2]
                    wstres = "wst%d" % (ci % 2)
                    S.dma("sp", out=wst[:, :, 0:ncol], in_=w_in_v[:, :, c0:c0 + ncol], writes=[wstres])
                    S.op("dve", lambda e, wb=wb, ncol=ncol, wst=wst: e.tensor_copy(out=wb[:, 0:8, 0:ncol], in_=wst[:, 0:8, 0:ncol]),
                         reads=[wstres], writes=[wres + "a"])
                    S.op("act", lambda e, wb=wb, ncol=ncol, wst=wst: e.activation(out=wb[:, 8:16, 0:ncol], in_=wst[:, 8:16, 0:ncol], func=ACT.Copy),
                         reads=[wstres], writes=[wres + "b"])
                    wr = [wres + "a", wres + "b"]
                    if kind == "tm":
                        for ti in range(NTG1):
                            t0 = g * GT1 + ti * 128
                            b = nbank()
                            for k in range(KD):
                                S.op("pe", lambda e, k=k, b=b, ti=ti, wb=wb: e.matmul(
                                    out=ps[b][:, :], lhsT=xnT[:, k, ti * 128:(ti + 1) * 128], rhs=wb[:, k, :],
                                    start=(k == 0), stop=(k == KD - 1)),
                                    reads=["xnT"] + wr, writes=["ps%d" % b], inc=(k == KD - 1))
                            si = nstg()
                            S.op("dve", lambda e, b=b, si=si, aux=aux: e.tensor_tensor(
                                out=stg[si][:], in0=ps[b][:, :], in1=bias_tm[:, aux:aux + 512], op=ALU.add),
                                reads=["ps%d" % b, "bias_tm"], writes=["stg%d" % si])
                            S.dma("pool", out=Ptm[t0:t0 + 128, aux:aux + 512], in_=stg[si][:], reads=["stg%d" % si])
                    elif kind in ("qk", "g"):
                        for bl in range(4):
                          for hv in range(HN):
                            b = nbank()
                            for k in range(KD):
                                S.op("pe", lambda e, k=k, b=b, bl=bl, wb=wb, hv=hv: e.matmul(
                                    out=ps[b][:, 0:HW], lhsT=wb[:, k, bl * 128:(bl + 1) * 128], rhs=xnT[:, k, hv * HW:(hv + 1) * HW],
                                    start=(k == 0), stop=(k == KD - 1)),
                                    reads=["xnT"] + wr, writes=["ps%d" % b], inc=(k == KD - 1))
                            si = nstg()
                            blk = aux + bl
                            c_lo = g * GT1 + hv * HW
                            if kind == "qk":
                                S.op("act", lambda e, b=b, si=si, blk=blk: e.activation(
                                    out=stg[si][:, 0:HW], in_=ps[b][:, 0:HW], func=ACT.Identity,
                                    bias=bcol_qk[:, blk:blk + 1], scale=1.0),
                                    reads=["ps%d" % b, "bcol_qk"], writes=["stg%d" % si])
                                S.dma("pool", out=Pqk[blk * 128:(blk + 1) * 128, c_lo:c_lo + HW],
                                      in_=stg[si][:, 0:HW], reads=["stg%d" % si])
                            else:
                                S.op("act", lambda e, b=b, si=si, blk=blk: e.activation(
                                    out=stg[si][:, 0:HW], in_=ps[b][:, 0:HW], func=ACT.Sigmoid,
                                    bias=bcol_g[:, blk:blk + 1], scale=1.0),
                                    reads=["ps%d" % b, "bcol_g"], writes=["stg%d" % si])
                                S.dma("pool", out=Pg[blk * 128:(blk + 1) * 128, c_lo:c_lo + HW],
                                      in_=stg[si][:, 0:HW], reads=["stg%d" % si])
                    else:
                        for half, bc in ((0, bcol_i), (1, bcol_f)):
                          for hv in range(HN):
                            b = nbank()
                            for k in range(KD):
                                S.op("pe", lambda e, k=k, b=b, half=half, wb=wb, hv=hv: e.matmul(
                                    out=ps[b][0:4, 0:HW], lhsT=wb[:, k, half * 4:half * 4 + 4], rhs=xnT[:, k, hv * HW:(hv + 1) * HW],
                                    start=(k == 0), stop=(k == KD - 1)),
                                    reads=["xnT"] + wr, writes=["ps%d" % b], inc=(k == KD - 1))
                            S.op("act", lambda e, b=b, bc=bc: e.activation(
                                out=stgif[:, :], in_=ps[b][0:4, 0:HW], func=ACT.Identity, bias=bc[:, 0:1], scale=1.0),
                                reads=["ps%d" % b, "bcol_i", "bcol_f"], writes=["stgif"])
                            c_lo = g * GT1 + hv * HW
                            S.dma("pool", out=Pif[half * 4:half * 4 + 4, c_lo:c_lo + HW], in_=stgif[:, :],
                                  reads=["stgif"])
            S.full_barrier()

    bank = [0]

    def nbank():
        b = bank[0]
        bank[0] = (b + 1) % 8
        return b

    def p2_record(sb2):
        Q = Rec()
        gq = sb2("gq", [128, 64], F32)
        gk = sb2("gk", [128, 64], F32)
        Q.dma("sp", out=gq[:], in_=q_norm_g[0:1, :].partition_broadcast(128), writes=["gq"])
        Q.dma("sp", out=gk[:], in_=k_norm_g[0:1, :].partition_broadcast(128), writes=["gk"])
        Q.op("dve", lambda e: e.tensor_scalar(out=gq[:], in0=gq[:], scalar1=0.125, scalar2=None, op0=ALU.mult),
             reads=["gq"], writes=["gq"])
        esk = sb2("esk", [64, 16], F32)
        Q.dma("sp", out=esk[:], in_=sinks[0:1, :].partition_broadcast(64), writes=["esk"])
        Q.op("act", lambda e: e.activation(out=esk[:], in_=esk[:], func=ACT.Exp), reads=["esk"], writes=["esk"])
        maskc = sb2("maskc", [128, 128], F32)
        maskp = sb2("maskp", [128, 128], F32)
        Q.op("pool", lambda e: e.memset(maskc[:], 1.0), writes=["maskc"])
        Q.op("pool", lambda e: e.affine_select(out=maskc[:], in_=maskc[:], compare_op=ALU.is_ge, fill=0.0,
                                                base=0, pattern=[[1, 128]], channel_multiplier=-1),
             reads=["maskc"], writes=["maskc"])
        Q.op("dve", lambda e: e.tensor_scalar(out=maskp[:], in0=maskc[:], scalar1=-1.0, scalar2=1.0,
                                              op0=ALU.mult, op1=ALU.add), reads=["maskc"], writes=["maskp"])
        ones_bf = sb2("ones_bf", [128, 64], BF16)
        Q.op("pool", lambda e: e.memset(ones_bf[:], 1.0), writes=["ones_bf"])
        qt = sb2("qt", [128, 1024], F32)
        kt = sb2("kt", [128, 256], F32)
        vt = sb2("vt", [128, 256], F32)
        sq = sb2("sq", [128, 1024], F32)
        ssq16 = sb2("ssq16", [128, 16], F32)
        ssq4 = sb2("ssq4", [128, 4], F32)
        qn = sb2("qn", [128, 1024], BF16)
        kn = sb2("kn", [128, 256], BF16)
        qT = sb2("qT", [64, 16, 128], BF16)
        kT = [sb2("kT%d" % i, [64, 4, 128], BF16) for i in range(2)]
        vb = [sb2("vb%d" % i, [128, 256], BF16) for i in range(2)]
        E = sb2("E", [128, 512], F32)
        PTc = sb2("PTc", [128, 512], BF16)
        PTp = sb2("PTp", [128, 512], BF16)
        den = sb2("den", [64, 512], F32)
        attT = sb2("attT", [64, 16, 128], BF16)
        AttT_v = AttT.rearrange("(h d) t -> d h t", d=64)

        def headnorm(src, nh, ssqt, gt, dst, sres, dres):
            w = nh * 64
            Q.op("dve", lambda e: e.tensor_tensor(out=sq[:, 0:w], in0=src[:, 0:w], in1=src[:, 0:w], op=ALU.mult),
                 reads=[sres], writes=["sq"])
            Q.op("dve", lambda e: e.tensor_reduce(out=ssqt[:], in_=sq[:, 0:w].rearrange("p (h d) -> p h d", d=64),
                                                  axis=AX.X, op=ALU.add), reads=["sq"], writes=["ssqt"])
            Q.op("dve", lambda e: e.tensor_scalar(out=ssqt[:], in0=ssqt[:], scalar1=1.0 / 64, scalar2=EPS,
                                                  op0=ALU.mult, op1=ALU.add), reads=["ssqt"], writes=["ssqt"])
            Q.op("act", lambda e: e.activation(out=ssqt[:], in_=ssqt[:], func=ACT.Sqrt), reads=["ssqt"], writes=["ssqt"])
            Q.op("dve", lambda e: e.reciprocal(out=ssqt[:], in_=ssqt[:]), reads=["ssqt"], writes=["ssqt"])
            Q.op("dve", lambda e: e.tensor_tensor(out=sq[:, 0:w].rearrange("p (h d) -> p h d", d=64),
                                                  in0=src[:, 0:w].rearrange("p (h d) -> p h d", d=64),
                                                  in1=ssqt[:].unsqueeze(2).to_broadcast([128, nh, 64]), op=ALU.mult),
                 reads=[sres, "ssqt"], writes=["sq"])
            Q.op("dve", lambda e: e.tensor_tensor(out=dst[:, 0:w].rearrange("p (h d) -> p h d", d=64),
                                                  in0=sq[:, 0:w].rearrange("p (h d) -> p h d", d=64),
                                                  in1=gt[:].unsqueeze(1).to_broadcast([128, nh, 64]), op=ALU.mult),
                 reads=["sq", "gq", "gk"], writes=[dres])

        for s_ in range(NSEQ):
            for qb in range(TPS):
                t0 = s_ * SEQ + qb * 128
                cur = qb % 2
                prv = 1 - cur
                Q.dma("sp", out=qt[:], in_=Ptm[t0:t0 + 128, 0:1024], writes=["qt"])
                Q.dma("sp", out=kt[:], in_=Ptm[t0:t0 + 128, 1024:1280], writes=["kt"])
                Q.dma("sp", out=vt[:], in_=Ptm[t0:t0 + 128, 1280:1536], writes=["vt"])
                headnorm(qt, 16, ssq16, gq, qn, "qt", "qn")
                headnorm(kt, 4, ssq4, gk, kn, "kt", "kn")
                Q.op("act", lambda e, cur=cur: e.activation(out=vb[cur][:], in_=vt[:], func=ACT.Copy),
                     reads=["vt"], writes=["vb%d" % cur])
                for half in range(2):
                    b = 6 + half
                    Q.begin()
                    for h8 in range(8):
                        h = half * 8 + h8
                        Q.op("pe", lambda e, b=b, h=h, h8=h8: e.transpose(
                            out=psb(b)[0:64, h8 * 128:(h8 + 1) * 128], in_=qn[:, h * 64:(h + 1) * 64], identity=identb[:]),
                            reads=["qn", "identb"], writes=["ps%d" % b], inc=(h8 == 7))
                    Q.end()
                    Q.op("act", lambda e, b=b, half=half: e.activation(
                        out=qT[:, half * 8:(half + 1) * 8, :],
                        in_=psb(b)[0:64, :].rearrange("p (h t) -> p h t", h=8), func=ACT.Copy),
                        reads=["ps%d" % b], writes=["qT"])
                b = 6
                Q.begin()
                for h in range(4):
                    Q.op("pe", lambda e, b=b, h=h: e.transpose(
                        out=psb(b)[0:64, h * 128:(h + 1) * 128], in_=kn[:, h * 64:(h + 1) * 64], identity=identb[:]),
                        reads=["kn", "identb"], writes=["ps%d" % b], inc=(h == 3))
                Q.end()
                Q.op("act", lambda e, b=b, cur=cur: e.activation(
                    out=kT[cur][:, :, :], in_=psb(b)[0:64, 0:512].rearrange("p (h t) -> p h t", h=4), func=ACT.Copy),
                    reads=["ps%d" % b], writes=["kT%d" % cur])
                for hk in range(4):
                    qTv = qT[:, 4 * hk:4 * hk + 4, :].rearrange("p h t -> p (h t)")
                    srcs = [(cur, maskc, PTc, "PTc")]
                    if qb > 0:
                        srcs.append((prv, maskp, PTp, "PTp"))
                    for (sl, mk, PT, pres) in srcs:
                        bS = 6
                        Q.op("pe", lambda e, bS=bS, sl=sl, hk=hk, qTv=qTv: e.matmul(
                            out=ps[bS][:, :], lhsT=kT[sl][:, hk, :], rhs=qTv, start=True, stop=True),
                            reads=["kT%d" % sl, "qT"], writes=["ps%d" % bS])
                        Q.op("act", lambda e, bS=bS: e.activation(out=E[:], in_=ps[bS][:, :], func=ACT.Exp),
                             reads=["ps%d" % bS], writes=["E"])
                        Q.op("dve", lambda e, mk=mk, PT=PT: e.tensor_tensor(
                            out=PT[:].rearrange("p (h t) -> p h t", h=4), in0=E[:].rearrange("p (h t) -> p h t", h=4),
                            in1=mk[:].unsqueeze(1).to_broadcast([128, 4, 128]), op=ALU.mult),
                            reads=["E", "maskc", "maskp"], writes=[pres])
                    bO = 7
                    bD = 6
                    Q.begin()
                    for i, (sl, mk, PT, pres) in enumerate(srcs):
                        Q.op("pe", lambda e, bO=bO, sl=sl, PT=PT, i=i, hk=hk: e.matmul(
                            out=ps[bO][0:64, :], lhsT=vb[sl][:, hk * 64:(hk + 1) * 64], rhs=PT[:],
                            start=(i == 0), stop=(i == len(srcs) - 1)),
                            reads=["vb%d" % sl, pres], writes=["ps%d" % bO])
                    for i, (sl, mk, PT, pres) in enumerate(srcs):
                        Q.op("pe", lambda e, bD=bD, PT=PT, i=i: e.matmul(
                            out=ps[bD][0:64, :], lhsT=ones_bf[:, 0:64], rhs=PT[:],
                            start=(i == 0), stop=(i == len(srcs) - 1)),
                            reads=["ones_bf", pres], writes=["ps%d" % bD])
                    Q.end()
                    Q.op("dve", lambda e, bD=bD, hk=hk: e.tensor_tensor(
                        out=den[:].rearrange("p (h t) -> p h t", h=4),
                        in0=ps[bD][0:64, :].rearrange("p (h t) -> p h t", h=4),
                        in1=esk[:, 4 * hk:4 * hk + 4].unsqueeze(2).to_broadcast([64, 4, 128]), op=ALU.add),
                        reads=["ps%d" % bD, "esk"], writes=["den"])
                    Q.op("dve", lambda e: e.reciprocal(out=den[:], in_=den[:]), reads=["den"], writes=["den"])
                    Q.op("dve", lambda e, bO=bO, hk=hk: e.tensor_tensor(
                        out=attT[:, 4 * hk:4 * hk + 4, :].rearrange("p h t -> p (h t)"),
                        in0=ps[bO][0:64, :], in1=den[:], op=ALU.mult),
                        reads=["ps%d" % bO, "den"], writes=["attT"])
                Q.dma("pool", out=AttT_v[:, :, t0:t0 + 128], in_=attT[:], reads=["attT"])
        return Q.steps

    p2_in_p3 = (2 in phases) and (3 in phases)
    if 2 in phases and not p2_in_p3:
        with ExitStack() as es2:
            def sb2(name, shape, dt):
                return es2.enter_context(nc.sbuf_tensor(uniq(name), list(shape), dt))
            replay(p2_record(sb2), 10 ** 9)
            S.full_barrier()

    if 3 in phases:
        with ExitStack() as es3:
            def sb3(name, shape, dt):
                return es3.enter_context(nc.sbuf_tensor(uniq(name), list(shape), dt))
            i4 = sb3("i4", [4, SEQ], F32)
            f4 = sb3("f4", [4, SEQ], F32)
            B4 = sb3("B4", [4, SEQ], F32)
            G4 = sb3("G4", [4, SEQ], F32)
            ones4 = sb3("ones4", [4, SEQ], F32)
            S.op("pool", lambda e: e.memset(ones4[:], 1.0), writes=["ones4"])
            sel = sb3("sel", [4, 4, 128], F32)
            S.op("pool", lambda e: e.memset(sel[:], 1.0), writes=["sel"])
            S.op("pool", lambda e: e.affine_select(out=sel[:], in_=sel[:], compare_op=ALU.is_equal, fill=0.0, base=0,
                                                    pattern=[[-1, 4], [0, 128]], channel_multiplier=1),
                 reads=["sel"], writes=["sel"])
            maskbd = sb3("maskbd", [128, 128], F32)
            S.op("pool", lambda e: e.memset(maskbd[:], 1.0), writes=["maskbd"])
            S.op("pool", lambda e: e.affine_select(out=maskbd[:], in_=maskbd[:], compare_op=ALU.is_ge, fill=0.0,
                                                    base=0, pattern=[[1, 128]], channel_multiplier=-1),
                 reads=["maskbd"], writes=["maskbd"])
            S.op("pool", lambda e: e.memset(maskbd[0:64, 64:128], 0.0), reads=["maskbd"], writes=["maskbd"])
            cw = sb3("cw", [128, 16, 4], F32)
            cb = sb3("cb", [128, 16], F32)
            for j in range(4):
                S.dma("sp", out=cw[:, :, j], in_=conv_w[j, :].rearrange("(b p) -> p b", p=128), writes=["cw"],
                      allow_slow_non_contiguous=True)
            S.dma("sp", out=cb[:], in_=conv_b[0, :].rearrange("(b p) -> p b", p=128), writes=["cb"],
                  allow_slow_non_contiguous=True)
            gml = sb3("gml", [128, 1024], F32)
            bload(gml[:], ml_norm_g[0:1, :], 1024, "gml")
            xin = sb3("xin", [128, 16, 131], F32)
            acc = sb3("acc", [128, 16, 128], F32)
            tmpc = sb3("tmpc", [128, 16, 128], F32)
            qTb = sb3("qTb", [128, 8, 128], BF16)
            kTb = sb3("kTb", [128, 8, 128], BF16)
            ktm = sb3("ktm", [128, 1024], BF16)
            vt3 = sb3("vt3", [128, 1024], F32)
            vaug = sb3("vaug", [128, 4, 257], BF16)
            S.op("pool", lambda e: e.memset(vaug[:], 1.0), writes=["vaug"])
            so = sb3("so", [128, 1024], F32)
            cols = sb3("cols", [128, 12], F32)
            emt = sb3("emt", [128, 4], F32)
            mprev = sb3("mprev", [128, 4], F32)
            argt = [sb3("argt%d" % i, [128, 128], F32) for i in range(4)]
            sctf = [sb3("sctf%d" % i, [128, 128], F32) for i in range(4)]
            sctb = [sb3("sctb%d" % i, [128, 128], BF16) for i in range(4)]
            wcol = [sb3("wcol%d" % i, [128, 1], F32) for i in range(4)]
            kw = [sb3("kw%d" % i, [128, 256], BF16) for i in range(4)]
            wbt = [sb3("wbt%d" % i, [128, 128], F32) for i in range(4)]
            qst = [sb3("qst%d" % i, [128, 2, 128], BF16) for i in range(4)]
            gendl = [sb3("gend%d" % i, [128, 2], F32) for i in range(4)]
            Cf = sb3("Cf", [128, 4, 2, 257], F32)
            Cb = sb3("Cb", [128, 4, 2, 257], BF16)
            dn = [sb3("dn%d" % i, [128, 1], F32) for i in range(4)]
            hh = [sb3("hh%d" % i, [128, 256], F32) for i in range(4)]
            junk3 = [sb3("junk3%d" % i, [128, 256], BF16) for i in range(4)]
            ssqh = [sb3("ssqh%d" % i, [128, 1], F32) for i in range(4)]
            hn = [sb3("hn%d" % i, [128, 256], F32) for i in range(4)]
            ml = sb3("ml", [128, 1024], BF16)
            mlT = sb3("mlT", [128, 8, 128], BF16)
            Pqk_v = Pqk.rearrange("(b p) t -> p b t", p=128)
            MlT_v = MlT.rearrange("(b p) t -> p b t", p=128)
            lb = [0]

            def lbank():
                lb[0] = (lb[0] + 1) % 4
                return lb[0]

            p0s = p0_record(sb3, nbuf=1) if p0_in_p3 else []
            p0_per_round = len(p0s) // (NT * 2 * 42) + 1
            p2s = p2_record(sb3) if p2_in_p3 else []
            p2_per_round = len(p2s) // (NT * 2 * 42) + 1

            for s_ in range(NSEQ):
                ts0 = s_ * SEQ
                S.dma("sp", out=i4[:], in_=Pif[0:4, ts0:ts0 + SEQ], writes=["i4"])
                S.dma("sp", out=f4[:], in_=Pif[4:8, ts0:ts0 + SEQ], writes=["f4"])
                S.op("act", lambda e: e.activation(out=f4[:], in_=f4[:], func=ACT.Exp, scale=-1.0), reads=["f4"], writes=["f4"])
                S.op("act", lambda e: e.activation(out=f4[:], in_=f4[:], func=ACT.Ln, bias=1.0), reads=["f4"], writes=["f4"])
                S.op("dve", lambda e: e.tensor_tensor_scan(out=B4[:], data0=ones4[:], data1=f4[:], initial=0.0,
                                                           op0=ALU.mult, op1=ALU.subtract),
                     reads=["ones4", "f4"], writes=["B4"])
                S.op("dve", lambda e: e.tensor_tensor(out=i4[:], in0=i4[:], in1=B4[:], op=ALU.subtract),
                     reads=["i4", "B4"], writes=["i4"])
                S.op("dve", lambda e: e.tensor_tensor_scan(out=G4[:], data0=i4[:], data1=i4[:], initial=-1e30,
                                                           op0=ALU.max, op1=ALU.max), reads=["i4"], writes=["G4"])
                S.op("pool", lambda e: e.memset(Cf[:], 0.0), writes=["Cf%d%d" % (a_, b_) for a_ in range(4) for b_ in range(2)])
                S.op("pool", lambda e: e.memset(Cb[:], 0.0), writes=["Cb%d%d" % (a_, b_) for a_ in range(4) for b_ in range(2)])
                S.op("pool", lambda e: e.memset(mprev[:], -1e30), writes=["mprev%d" % a_ for a_ in range(4)])
                for tb in range(TPS):
                    t0 = ts0 + tb * 128
                    tl = tb * 128
                    b = lbank()
                    for qi, (src, sres) in enumerate(((i4, "i4"), (G4, "G4"), (B4, "B4"))):
                        S.op("pe", lambda e, b=b, qi=qi, src=src: e.transpose(
                            out=ps[b][:, qi * 4:qi * 4 + 4], in_=src[0:4, tl:tl + 128], identity=identf[0:4, 0:4]),
                            reads=[sres, "identf"], writes=["ps%d" % b])
                    S.op("dve", lambda e, b=b: e.tensor_copy(out=cols[:], in_=ps[b][:, 0:12]), reads=["ps%d" % b], writes=["cols"])
                    S.op("dve", lambda e: e.tensor_tensor(out=emt[:], in0=cols[:, 4:8], in1=cols[:, 8:12], op=ALU.add),
                         reads=["cols"], writes=["emt"])
                    S.op("act", lambda e: e.activation(out=emt[:], in_=emt[:], func=ACT.Exp, scale=-1.0), reads=["emt"], writes=["emt"])
                    if tb == 0:
                        S.op("pool", lambda e: e.memset(xin[:, :, 0:3], 0.0), reads=["xin"], writes=["xin"])
                    else:
                        S.op("dve", lambda e: e.tensor_copy(out=xin[:, :, 0:3], in_=xin[:, :, 128:131]), reads=["xin"], writes=["xin"])
                    S.dma("sp", out=xin[:, :, 3:131], in_=Pqk_v[:, :, t0:t0 + 128], reads=["xin"], writes=["xin"])
                    S.op("dve", lambda e: e.tensor_tensor(out=acc[:], in0=xin[:, :, 0:128],
                                                          in1=cw[:, :, 0:1].to_broadcast([128, 16, 128]), op=ALU.mult),
                         reads=["xin", "cw"], writes=["acc"])
                    for j in range(1, 4):
                        S.op("dve", lambda e, j=j: e.tensor_tensor(out=tmpc[:], in0=xin[:, :, j:j + 128],
                                                                   in1=cw[:, :, j:j + 1].to_broadcast([128, 16, 128]), op=ALU.mult),
                             reads=["xin", "cw"], writes=["tmpc"])
                        S.op("dve", lambda e: e.tensor_tensor(out=acc[:], in0=acc[:], in1=tmpc[:], op=ALU.add),
                             reads=["acc", "tmpc"], writes=["acc"])
                    S.op("dve", lambda e: e.tensor_tensor(out=acc[:], in0=acc[:],
                                                          in1=cb[:].unsqueeze(2).to_broadcast([128, 16, 128]), op=ALU.add),
                         reads=["acc", "cb"], writes=["acc"])
                    S.op("act", lambda e: e.activation(out=acc[:], in_=acc[:], func=ACT.Silu), reads=["acc"], writes=["acc"])
                    S.op("dve", lambda e: e.tensor_copy(out=qTb[:], in_=acc[:, 0:8, :]), reads=["acc"], writes=["qTb"])
                    S.op("dve", lambda e: e.tensor_scalar(out=kTb[:], in0=acc[:, 8:16, :], scalar1=1.0 / 16, scalar2=None,
                                                          op0=ALU.mult), reads=["acc"], writes=["kTb"])
                    b = lbank()
                    for blk in range(8):
                        S.op("pe", lambda e, b=b, blk=blk: e.transpose(
                            out=psb(b)[:, blk * 128:(blk + 1) * 128], in_=kTb[:, blk, :], identity=identb[:]),
                            reads=["kTb", "identb"], writes=["ps%d" % b], inc=(blk == 7))
                    S.op("act", lambda e, b=b: e.activation(out=ktm[:], in_=psb(b)[:, :], func=ACT.Copy),
                         reads=["ps%d" % b], writes=["ktm"])
                    S.dma("sp", out=vt3[:], in_=Ptm[t0:t0 + 128, 1536:2560], writes=["vt3"])
                    S.op("act", lambda e: e.activation(out=vaug[:, :, 0:256], in_=vt3[:].rearrange("p (h d) -> p h d", h=4),
                                                       func=ACT.Copy), reads=["vt3"], writes=["vaug"])
                    S.dma("sp", out=so[:], in_=Ptm[t0:t0 + 128, 2560:3584], writes=["so"])
                    S.op("act", lambda e: e.activation(out=so[:], in_=so[:], func=ACT.Sigmoid), reads=["so"], writes=["so"])
                    def head_chain(h):
                        Q = Rec()
                        H = str(h)
                        bX = h % 2
                        bG = bX
                        Q.op("pe", lambda e: e.matmul(out=ps[bG][:, 0:128], lhsT=sel[0:4, h, :],
                                                      rhs=G4[0:4, tl:tl + 128], start=True, stop=True),
                             reads=["sel", "G4"], writes=["ps%d" % bG])
                        bS = bX
                        Q.begin()
                        for kc in range(2):
                            Q.op("pe", lambda e, kc=kc: e.matmul(
                                out=ps[bS][:, 128:256], lhsT=kTb[:, h * 2 + kc, :], rhs=qTb[:, h * 2 + kc, :],
                                start=(kc == 0), stop=(kc == 1)), reads=["kTb", "qTb"], writes=["ps%d" % bS])
                        Q.end()
                        Q.op("dve", lambda e: e.tensor_scalar(
                            out=argt[h][:], in0=ps[bG][:, 0:128], scalar1=cols[:, h:h + 1], scalar2=0.0,
                            op0=ALU.subtract, op1=ALU.max), reads=["ps%d" % bG, "cols"], writes=["argt" + H])
                        Q.op("dve", lambda e: e.tensor_copy(out=gendl[h][:, 0:1], in_=ps[bG][:, 63:64]),
                             reads=["ps%d" % bG], writes=["gend" + H])
                        Q.op("dve", lambda e: e.tensor_copy(out=gendl[h][:, 1:2], in_=ps[bG][:, 127:128]),
                             reads=["ps%d" % bG, "gend" + H], writes=["gend" + H])
                        Q.op("dve", lambda e: e.tensor_scalar(
                            out=wbt[h][:, 0:64], in0=ps[bG][:, 0:64], scalar1=mprev[:, h:h + 1], scalar2=80.0,
                            op0=ALU.subtract, op1=ALU.min), reads=["ps%d" % bG, "mprev" + H], writes=["wbt" + H])
                        Q.op("dve", lambda e: e.tensor_scalar(
                            out=wbt[h][:, 64:128], in0=ps[bG][:, 64:128], scalar1=gendl[h][:, 0:1], scalar2=80.0,
                            op0=ALU.subtract, op1=ALU.min), reads=["ps%d" % bG, "gend" + H, "wbt" + H], writes=["wbt" + H])
                        Q.op("act", lambda e: e.activation(out=argt[h][:], in_=argt[h][:], func=ACT.Exp, scale=-1.0),
                             reads=["argt" + H], writes=["argt" + H])
                        Q.op("act", lambda e: e.activation(out=wbt[h][:], in_=wbt[h][:], func=ACT.Exp, scale=-1.0),
                             reads=["wbt" + H], writes=["wbt" + H])
                        for c in range(2):
                            Q.op("act", lambda e, c=c: e.activation(
                                out=wcol[h][64 * c:64 * c + 64, 0:1], in_=gendl[h][64 * c:64 * c + 64, c:c + 1], func=ACT.Exp,
                                scale=-1.0, bias=cols[64 * c:64 * c + 64, h:h + 1]),
                                reads=["gend" + H, "cols", "wcol" + H], writes=["wcol" + H])
                        Q.op("dve", lambda e: e.tensor_copy(out=mprev[:, h:h + 1], in_=gendl[h][:, 1:2]),
                             reads=["gend" + H, "mprev" + H], writes=["mprev" + H])
                        Q.op("dve", lambda e: e.tensor_tensor(out=sctf[h][:], in0=ps[bS][:, 128:256], in1=argt[h][:], op=ALU.mult),
                             reads=["ps%d" % bS, "argt" + H], writes=["sctf" + H])
                        Q.op("dve", lambda e: e.tensor_tensor(out=sctb[h][:], in0=sctf[h][:], in1=maskbd[:], op=ALU.mult),
                             reads=["sctf" + H, "maskbd"], writes=["sctb" + H])
                        Q.op("dve", lambda e: e.tensor_scalar(out=kw[h][:], in0=ktm[:, h * 256:(h + 1) * 256],
                                                              scalar1=wcol[h][:, 0:1], scalar2=None, op0=ALU.mult),
                             reads=["ktm", "wcol" + H], writes=["kw" + H])
                        Q.op("dve", lambda e: e.tensor_tensor(
                            out=qst[h][:], in0=qTb[:, 2 * h:2 * h + 2, :], in1=wbt[h][:].unsqueeze(1).to_broadcast([128, 2, 128]),
                            op=ALU.mult), reads=["qTb", "wbt" + H], writes=["qst" + H])
                        bN = 2 + h % 2
                        Q.op("pe", lambda e: e.matmul(out=ps[bN][:, 0:257], lhsT=sctb[h][:], rhs=vaug[:, h, :],
                                                      start=True, stop=False),
                             reads=["sctb" + H, "vaug"], writes=["ps%d" % bN])
                        for c in range(2):
                            Q.begin()
                            for kc in range(2):
                                Q.op("pe", lambda e, c=c, kc=kc: e.matmul(
                                    out=ps[bN][64 * c:64 * c + 64, 0:257], lhsT=qst[h][:, kc, 64 * c:64 * c + 64],
                                    rhs=Cb[:, h, kc, :], start=False, stop=(c == 1 and kc == 1)),
                                    reads=["qst" + H, "Cb%d%d" % (h, kc)], writes=["ps%d" % bN])
                            Q.end()
                            for kc in range(2):
                                bU = bX
                                Q.op("pe", lambda e, bU=bU, c=c, kc=kc: e.matmul(
                                    out=ps[bU][:, 0:257], lhsT=kw[h][64 * c:64 * c + 64, kc * 128:(kc + 1) * 128],
                                    rhs=vaug[64 * c:64 * c + 64, h, :], start=True, stop=True),
                                    reads=["kw" + H, "vaug"], writes=["ps%d" % bU])
                                Q.op("dve", lambda e, bU=bU, c=c, kc=kc: e.scalar_tensor_tensor(
                                    out=Cf[:, h, kc, :], in0=Cf[:, h, kc, :], scalar=wbt[h][:, 64 * c + 63:64 * c + 64],
                                    in1=ps[bU][:, 0:257], op0=ALU.mult, op1=ALU.add),
                                    reads=["Cf%d%d" % (h, kc), "wbt" + H, "ps%d" % bU], writes=["Cf%d%d" % (h, kc)])
                                Q.op("act", lambda e, kc=kc: e.activation(out=Cb[:, h, kc, :], in_=Cf[:, h, kc, :], func=ACT.Copy),
                                     reads=["Cf%d%d" % (h, kc)], writes=["Cb%d%d" % (h, kc)])
                        Q.op("act", lambda e: e.activation(out=dn[h][:], in_=ps[bN][:, 256:257], func=ACT.Abs),
                             reads=["ps%d" % bN], writes=["dn" + H])
                        Q.op("dve", lambda e: e.tensor_tensor(out=dn[h][:], in0=dn[h][:], in1=emt[:, h:h + 1], op=ALU.max),
                             reads=["dn" + H, "emt"], writes=["dn" + H])
                        Q.op("dve", lambda e: e.reciprocal(out=dn[h][:], in_=dn[h][:]), reads=["dn" + H], writes=["dn" + H])
                        Q.op("act", lambda e: e.activation(out=hh[h][:], in_=ps[bN][:, 0:256], func=ACT.Copy, scale=dn[h][:, 0:1]),
                             reads=["ps%d" % bN, "dn" + H], writes=["hh" + H])
                        Q.op("act", lambda e: e.activation(out=junk3[h][:], in_=hh[h][:], func=ACT.Square, accum_out=ssqh[h][:]),
                             reads=["hh" + H], writes=["junk3" + H, "ssqh" + H])
                        Q.op("dve", lambda e: e.tensor_scalar(out=ssqh[h][:], in0=ssqh[h][:], scalar1=1.0 / 256, scalar2=EPS,
                                                              op0=ALU.mult, op1=ALU.add), reads=["ssqh" + H], writes=["ssqh" + H])
                        Q.op("act", lambda e: e.activation(out=ssqh[h][:], in_=ssqh[h][:], func=ACT.Sqrt),
                             reads=["ssqh" + H], writes=["ssqh" + H])
                        Q.op("dve", lambda e: e.reciprocal(out=ssqh[h][:], in_=ssqh[h][:]), reads=["ssqh" + H], writes=["ssqh" + H])
                        Q.op("dve", lambda e: e.scalar_tensor_tensor(
                            out=hn[h][:], in0=hh[h][:], scalar=ssqh[h][:, 0:1], in1=gml[:, h * 256:(h + 1) * 256],
                            op0=ALU.mult, op1=ALU.mult), reads=["hh" + H, "ssqh" + H, "gml"], writes=["hn" + H])
                        Q.op("dve", lambda e: e.tensor_tensor(out=ml[:, h * 256:(h + 1) * 256], in0=hn[h][:],
                                                              in1=so[:, h * 256:(h + 1) * 256], op=ALU.mult),
                             reads=["hn" + H, "so"], writes=["ml" + H])
                        return Q.steps

                    for pair in ((0, 1), (2, 3)):
                        chains = [head_chain(h) for h in pair]
                        while any(chains):
                            for ch_ in chains:
                                replay(ch_, 1)
                            replay(p0s, p0_per_round)
                            replay(p2s, p2_per_round)
                    b = lbank()
                    for blk in range(8):
                        S.op("pe", lambda e, b=b, blk=blk: e.transpose(
                            out=psb(b)[:, blk * 128:(blk + 1) * 128], in_=ml[:, blk * 128:(blk + 1) * 128], identity=identb[:]),
                            reads=["ml%d" % (blk // 2), "identb"], writes=["ps%d" % b], inc=(blk == 7))
                    S.op("act", lambda e, b=b: e.activation(out=mlT[:].rearrange("p b t -> p (b t)"), in_=psb(b)[:, :], func=ACT.Copy),
                         reads=["ps%d" % b], writes=["mlT"])
                    S.dma("pool", out=MlT_v[:, :, t0:t0 + 128], in_=mlT[:], reads=["mlT"])
            replay(p0s, 10 ** 9)
            replay(p2s, 10 ** 9)
            S.full_barrier()

    if 4 in phases:
        with ExitStack() as es4:
            def sb4(name, shape, dt):
                return es4.enter_context(nc.sbuf_tensor(uniq(name), list(shape), dt))
            wstgs = [sb4("wstg%d" % i, [128, D], F32) for i in range(2)]
            wpa = sb4("wpa", [128, 8, D], BF16)
            wpm = sb4("wpm", [128, 8, D], BF16)
            for (wsrc, wdst, wres) in ((w_proj_att, wpa, "wpa"), (w_proj_ml, wpm, "wpm")):
                wv = wsrc.rearrange("(k p) n -> p k n", p=128)
                for k in range(8):
                    wstg = wstgs[k % 2]
                    S.dma("sp", out=wstg[:], in_=wv[:, k, :], writes=["wstg%d" % (k % 2)])
                    if k % 2 == 0:
                        S.op("dve", lambda e, k=k, wdst=wdst, wstg=wstg: e.tensor_copy(out=wdst[:, k, :], in_=wstg[:]),
                             reads=["wstg0"], writes=[wres + "e"])
                    else:
                        S.op("act", lambda e, k=k, wdst=wdst, wstg=wstg: e.activation(out=wdst[:, k, :], in_=wstg[:], func=ACT.Copy),
                             reads=["wstg1"], writes=[wres + "o"])
            attg = sb4("attg", [128, 8, GT], BF16)
            mlg = sb4("mlg", [128, 8, GT], BF16)
            sg0s = [sb4("sg0_%d" % i, [128, GT], F32) for i in range(2)]
            sg1s = [sb4("sg1_%d" % i, [128, GT], F32) for i in range(2)]
            t1 = sb4("t1", [128, GT], F32)
            t2 = sb4("t2", [128, GT], F32)
            mrg = [sb4("mrg%d" % i, [128, GT], BF16) for i in range(2)]
            AttT_k = AttT.rearrange("(k p) t -> p k t", p=128)
            MlT_k = MlT.rearrange("(k p) t -> p k t", p=128)
            for g in range(NG):
                S.dma("sp", out=attg[:], in_=AttT_k[:, :, g * GT:(g + 1) * GT], writes=["attg"])
                S.dma("sp", out=mlg[:], in_=MlT_k[:, :, g * GT:(g + 1) * GT], writes=["mlg"])
                for nb in range(16):
                    bA = nbank()
                    for kc in range(8):
                        S.op("pe", lambda e, bA=bA, kc=kc, nb=nb: e.matmul(
                            out=ps[bA][:, 0:GT], lhsT=wpa[:, kc, nb * 128:(nb + 1) * 128], rhs=attg[:, kc, :],
                            start=(kc == 0), stop=(kc == 7)), reads=["wpae", "wpao", "attg"], writes=["ps%d" % bA], inc=(kc == 7))
                    bM = nbank()
                    for kc in range(8):
                        S.op("pe", lambda e, bM=bM, kc=kc, nb=nb: e.matmul(
                            out=ps[bM][:, 0:GT], lhsT=wpm[:, kc, nb * 128:(nb + 1) * 128], rhs=mlg[:, kc, :],
                            start=(kc == 0), stop=(kc == 7)), reads=["wpme", "wpmo", "mlg"], writes=["ps%d" % bM], inc=(kc == 7))
                    sg0, sg1 = sg0s[nb % 2], sg1s[nb % 2]
                    S.dma("sp", out=sg0[:], in_=Pg[nb * 128:(nb + 1) * 128, g * GT:(g + 1) * GT], writes=["sg0_%d" % (nb % 2)])
                    S.dma("sp", out=sg1[:], in_=Pg[2048 + nb * 128:2048 + (nb + 1) * 128, g * GT:(g + 1) * GT], writes=["sg1_%d" % (nb % 2)])
                    S.op("dve", lambda e, bA=bA, sg0=sg0: e.tensor_tensor(out=t1[:], in0=ps[bA][:, 0:GT], in1=sg0[:], op=ALU.mult),
                         reads=["ps%d" % bA, "sg0_%d" % (nb % 2)], writes=["t1"])
                    S.op("dve", lambda e, bM=bM, sg1=sg1: e.tensor_tensor(out=t2[:], in0=ps[bM][:, 0:GT], in1=sg1[:], op=ALU.mult),
                         reads=["ps%d" % bM, "sg1_%d" % (nb % 2)], writes=["t2"])
                    mi = nb % 2
                    S.op("dve", lambda e, mi=mi: e.tensor_tensor(out=mrg[mi][:], in0=t1[:], in1=t2[:], op=ALU.add),
                         reads=["t1", "t2"], writes=["mrg%d" % mi])
                    S.dma("pool", out=MgT[nb * 128:(nb + 1) * 128, g * GT:(g + 1) * GT], in_=mrg[mi][:], reads=["mrg%d" % mi])
            S.full_barrier()
        with ExitStack() as es4:
            def sb4(name, shape, dt):
                return es4.enter_context(nc.sbuf_tensor(uniq(name), list(shape), dt))
            wstgs = [sb4("wstg2_%d" % i, [128, D], F32) for i in range(2)]
            wo = sb4("wo", [128, KD, D], BF16)
            wv = w_out.rearrange("(k p) n -> p k n", p=128)
            for k in range(KD):
                wstg = wstgs[k % 2]
                S.dma("sp", out=wstg[:], in_=wv[:, k, :], writes=["wstg2_%d" % (k % 2)])
                if k % 2 == 0:
                    S.op("dve", lambda e, k=k, wstg=wstg: e.tensor_copy(out=wo[:, k, :], in_=wstg[:]), reads=["wstg2_0"], writes=["woe"])
                else:
                    S.op("act", lambda e, k=k, wstg=wstg: e.activation(out=wo[:, k, :], in_=wstg[:], func=ACT.Copy), reads=["wstg2_1"], writes=["woo"])
            mT = sb4("mT", [128, KD, GT], BF16)
            xt4 = sb4("xt4", [128, D], F32)
            x1t = sb4("x1t", [128, D], F32)
            MgT_k = MgT.rearrange("(k p) t -> p k t", p=128)
            for g in range(NG):
                S.dma("sp", out=mT[:], in_=MgT_k[:, :, g * GT:(g + 1) * GT], writes=["mT"])
                for ti in range(NTG):
                    t0 = g * GT + ti * 128
                    S.dma("sp", out=xt4[:], in_=x[t0:t0 + 128, :], writes=["xt4"])
                    for ncn in range(4):
                        bO = nbank()
                        for kc in range(KD):
                            S.op("pe", lambda e, bO=bO, kc=kc, ti=ti, ncn=ncn: e.matmul(
                                out=ps[bO][:, :], lhsT=mT[:, kc, ti * 128:(ti + 1) * 128], rhs=wo[:, kc, ncn * 512:(ncn + 1) * 512],
                                start=(kc == 0), stop=(kc == KD - 1)), reads=["mT", "woe", "woo"], writes=["ps%d" % bO], inc=(kc == KD - 1))
                        S.op("dve", lambda e, bO=bO, ncn=ncn: e.tensor_tensor(
                            out=x1t[:, ncn * 512:(ncn + 1) * 512], in0=ps[bO][:, :], in1=xt4[:, ncn * 512:(ncn + 1) * 512],
                            op=ALU.add), reads=["ps%d" % bO, "xt4"], writes=["x1t"])
                    S.dma("pool", out=X1[t0:t0 + 128, :], in_=x1t[:], reads=["x1t"])
            S.full_barrier()

    if 5 in phases:
        TGn = min(512, T)
        NS = TGn // 128
        NTG5 = T // TGn
        CG = 4
        NGR = 128 // CG
        TB = min(64, TGn)
        Gd2 = [dscr("Gd%d" % i, [NGR, 128, TGn, CG], BF16) for i in range(2)]
        with ExitStack() as es5:
            def sb5(name, shape, dt):
                return es5.enter_context(nc.sbuf_tensor(uniq(name), list(shape), dt))
            gffn = sb5("gffn", [128, D], F32)
            bload(gffn[:], norm_ffn_g[0:1, :], D, "gffn")
            keysT = sb5("keysT", [128, 16, 128], F32)
            iota_i = sb5("iota_i", [128, 128], I32)
            iota128 = sb5("iota128", [128, 128], F32)
            S.op("pool", lambda e: e.iota(out=iota_i[:], pattern=[[1, 128]], base=0, channel_multiplier=0), writes=["iota_i"])
            S.op("dve", lambda e: e.tensor_copy(out=iota128[:], in_=iota_i[:]), reads=["iota_i"], writes=["iota128"])
            iota128b = sb5("iota128b", [128, 128], BF16)
            S.op("dve", lambda e: e.tensor_copy(out=iota128b[:], in_=iota_i[:]), reads=["iota_i"], writes=["iota128b"])
            xn2Ts = [sb5("xn2T%d" % i, [128, KD, TGn], BF16) for i in range(2)]
            iTs = [sb5("iT%d" % i, [128, TGn], F32) for i in range(2)]
            jTs = [sb5("jT%d" % i, [128, TGn], F32) for i in range(2)]
            gTs = [sb5("gT%d" % i, [128, TGn], F32) for i in range(2)]
            ohj = [sb5("ohj%d" % i, [128, 128], BF16) for i in range(8)]
            ohi = [sb5("ohi%d" % i, [128, 128], BF16) for i in range(8)]
            gst = [sb5("gst%d" % i, [128, NGR, TB, CG], BF16) for i in range(2)]
            with nc.sbuf_tensor(uniq("kst"), [128, 16, 128], F32) as kst:
                S.dma("sp", out=kst[:], in_=peer_keys.rearrange("b n d -> n b d"), writes=["kst"])
                for blk in range(16):
                    b = nbank()
                    S.op("pe", lambda e, b=b, blk=blk: e.transpose(out=ps[b][:, 0:128], in_=kst[:, blk, :], identity=identf[:]),
                         reads=["kst", "identf"], writes=["ps%d" % b])
                    S.op("act", lambda e, b=b, blk=blk: e.activation(out=keysT[:, blk, :], in_=ps[b][:, 0:128], func=ACT.Copy),
                         reads=["ps%d" % b], writes=["keysT"])
                S.full_barrier()

            def routing_scope(tg):
                with ExitStack() as esr:
                    def sbr(name, shape, dt):
                        return esr.enter_context(nc.sbuf_tensor(uniq(name), list(shape), dt))
                    par = tg % 2
                    xn2T, iT, jT, gT = xn2Ts[par], iTs[par], jTs[par], gTs[par]
                    bufsets = []
                    for ci_ in range(2):
                        x1t = sbr("x1t5", [128, D], F32)
                        xn2 = sbr("xn2", [128, D], BF16)
                        ssq5 = sbr("ssq5", [128, 1], F32)
                        wqb = [sbr("wqb%d" % i, [128, KD, 128], BF16) for i in range(2)]
                        qpb = [sbr("qpb%d" % i, [128, 128], F32) for i in range(2)]
                        stop = sbr("stop", [128, 16, 16], F32)
                        itop = sbr("itop", [128, 16, 16], U32)
                        tmpb = sbr("tmpb", [128, 128], F32)
                        cand = sbr("cand", [128, 8, 256], F32)
                        tmpc2 = sbr("tmpc2", [128, 256], F32)
                        ctop = sbr("ctop", [128, 8, 16], F32)
                        cpos = sbr("cpos", [128, 8, 16], U32)
                        gz = sbr("gz", [128, 8], F32)
                        au = sbr("au", [128, 8, 16], U32)
                        bu = sbr("bu", [128, 8, 16], U32)
                        af_ = sbr("af_", [128, 8, 16], F32)
                        bf_ = sbr("bf_", [128, 8, 16], F32)
                        itf = sbr("itf", [128, 16, 16], F32)
                        eq = sbr("eq", [128, 8, 16, 16], F32)
                        iidx = sbr("iidx", [128, 8, 16], F32)
                        jidx = sbr("jidx", [128, 8, 16], F32)
                        stop4 = stop[:].rearrange("p (h c) r -> p h c r", c=2)
                        itf4 = itf[:].rearrange("p (h c) r -> p h c r", c=2)
                        bufsets.append(dict(x1t=x1t, xn2=xn2, ssq5=ssq5, wqb=wqb, qpb=qpb, stop=stop, itop=itop, tmpb=tmpb, cand=cand, tmpc2=tmpc2, ctop=ctop, cpos=cpos, gz=gz, au=au, bu=bu, af_=af_, bf_=bf_, itf=itf, eq=eq, iidx=iidx, jidx=jidx, stop4=stop4, itf4=itf4))
                    SHARED = ("identb", "identf", "gffn", "keysT", "iota128", "iota128b")

                    def tile_chain(s_, ci):
                        Q = Rec()
                        bs_ = bufsets[ci]
                        x1t = bs_["x1t"]
                        xn2 = bs_["xn2"]
                        ssq5 = bs_["ssq5"]
                        wqb = bs_["wqb"]
                        qpb = bs_["qpb"]
                        stop = bs_["stop"]
                        itop = bs_["itop"]
                        tmpb = bs_["tmpb"]
                        cand = bs_["cand"]
                        tmpc2 = bs_["tmpc2"]
                        ctop = bs_["ctop"]
                        cpos = bs_["cpos"]
                        gz = bs_["gz"]
                        au = bs_["au"]
                        bu = bs_["bu"]
                        af_ = bs_["af_"]
                        bf_ = bs_["bf_"]
                        itf = bs_["itf"]
                        eq = bs_["eq"]
                        iidx = bs_["iidx"]
                        jidx = bs_["jidx"]
                        stop4 = bs_["stop4"]
                        itf4 = bs_["itf4"]
                        cb_ = [0]

                        def cbank():
                            cb_[0] = (cb_[0] + 1) % 4
                            return 4 * ci + cb_[0]

                        def ren(n):
                            if n in SHARED or n.startswith("ps"):
                                return n
                            if n in ("xn2T", "iT", "jT", "gT"):
                                return "%s%d_s%d" % (n, par, s_)
                            return "%s_c%d" % (n, ci)

                        class QQ:
                            @staticmethod
                            def op(e, fn, reads=(), writes=(), inc=True):
                                Q.op(e, fn, reads=[ren(r) for r in reads], writes=[ren(w) for w in writes], inc=inc)

                            @staticmethod
                            def dma(q, out, in_, reads=(), writes=(), **kw):
                                Q.dma(q, out=out, in_=in_, reads=[ren(r) for r in reads], writes=[ren(w) for w in writes], **kw)

                        t0 = tg * TGn + s_ * 128
                        QQ.dma("sp", out=x1t[:], in_=X1[t0:t0 + 128, :], writes=["x1t5"])
                        QQ.op("act", lambda e: e.activation(out=xn2[:], in_=x1t[:], func=ACT.Square, accum_out=ssq5[:]),
                             reads=["x1t5"], writes=["xn2", "ssq5"])
                        QQ.op("dve", lambda e: e.tensor_scalar(out=ssq5[:], in0=ssq5[:], scalar1=1.0 / D, scalar2=EPS,
                                                              op0=ALU.mult, op1=ALU.add), reads=["ssq5"], writes=["ssq5"])
                        QQ.op("act", lambda e: e.activation(out=ssq5[:], in_=ssq5[:], func=ACT.Sqrt), reads=["ssq5"], writes=["ssq5"])
                        QQ.op("dve", lambda e: e.reciprocal(out=ssq5[:], in_=ssq5[:]), reads=["ssq5"], writes=["ssq5"])
                        QQ.op("dve", lambda e: e.scalar_tensor_tensor(out=xn2[:], in0=x1t[:], scalar=ssq5[:, 0:1], in1=gffn[:],
                                                                     op0=ALU.mult, op1=ALU.mult),
                             reads=["x1t5", "ssq5", "gffn"], writes=["xn2"])
                        for kq in range(4):
                            b = cbank()
                            Q.begin()
                            for j in range(4):
                                k = kq * 4 + j
                                QQ.op("pe", lambda e, k=k, j=j, b=b: e.transpose(out=psb(b)[:, j * 128:(j + 1) * 128],
                                                                                in_=xn2[:, k * 128:(k + 1) * 128],
                                                                                identity=identb[:]),
                                     reads=["xn2", "identb"], writes=["ps%d" % b], inc=(j == 3))
                            Q.end()
                            QQ.op("act", lambda e, kq=kq, b=b, s_=s_: e.activation(
                                out=xn2T[:, kq * 4:(kq + 1) * 4, s_ * 128:(s_ + 1) * 128],
                                in_=psb(b)[:, 0:512].rearrange("p (j t) -> p j t", j=4), func=ACT.Copy),
                                reads=["ps%d" % b], writes=["xn2T"])
                        for blk in range(16):
                            sl = blk % 2
                            QQ.dma("sp", out=wqb[sl][:].rearrange("p k c -> p (k c)"), in_=WQs[blk].rearrange("p k c -> p (k c)"), writes=["wqb%d" % sl])
                            bq = cbank()
                            Q.begin()
                            for k in range(KD):
                                QQ.op("pe", lambda e, bq=bq, k=k, sl=sl, s_=s_: e.matmul(
                                    out=ps[bq][:, 0:128], lhsT=wqb[sl][:, k, :], rhs=xn2T[:, k, s_ * 128:(s_ + 1) * 128],
                                    start=(k == 0), stop=(k == KD - 1)), reads=["wqb%d" % sl, "xn2T"], writes=["ps%d" % bq], inc=(k == KD - 1))
                            Q.end()
                            QQ.op("act", lambda e, bq=bq, sl=sl: e.activation(out=qpb[sl][:], in_=ps[bq][:, 0:128], func=ACT.Copy),
                                 reads=["ps%d" % bq], writes=["qpb%d" % sl])
                            bs = cbank()
                            QQ.op("pe", lambda e, bs=bs, sl=sl, blk=blk: e.matmul(
                                out=ps[bs][:, 0:128], lhsT=qpb[sl][:], rhs=keysT[:, blk, :], start=True, stop=True),
                                reads=["qpb%d" % sl, "keysT"], writes=["ps%d" % bs])
                            pr = "ps%d" % bs
                            QQ.op("dve", lambda e, bs=bs, blk=blk: e.max(out=stop[:, blk, 0:8], in_=ps[bs][:, 0:128]),
                                 reads=[pr], writes=["stop"])
                            QQ.op("dve", lambda e, bs=bs, blk=blk: e.max_index(out=itop[:, blk, 0:8], in_max=stop[:, blk, 0:8],
                                                                              in_values=ps[bs][:, 0:128]),
                                 reads=[pr, "stop"], writes=["itop"])
                            QQ.op("dve", lambda e, bs=bs, blk=blk: e.match_replace(out=tmpb[:], in_to_replace=stop[:, blk, 0:8],
                                                                                  in_values=ps[bs][:, 0:128], imm_value=-1e30),
                                 reads=[pr, "stop"], writes=["tmpb"])
                            QQ.op("dve", lambda e, blk=blk: e.max(out=stop[:, blk, 8:16], in_=tmpb[:]), reads=["tmpb"], writes=["stop"])
                            QQ.op("dve", lambda e, blk=blk: e.max_index(out=itop[:, blk, 8:16], in_max=stop[:, blk, 8:16],
                                                                       in_values=tmpb[:]), reads=["tmpb", "stop"], writes=["itop"])
                        QQ.op("dve", lambda e: e.tensor_tensor(
                            out=cand[:].rearrange("p h (a b) -> p h a b", a=16),
                            in0=stop4[:, :, 0, :].unsqueeze(3).to_broadcast([128, 8, 16, 16]),
                            in1=stop4[:, :, 1, :].unsqueeze(2).to_broadcast([128, 8, 16, 16]), op=ALU.add),
                            reads=["stop"], writes=["cand"])
                        for h in range(8):
                            QQ.op("dve", lambda e, h=h: e.max(out=ctop[:, h, 0:8], in_=cand[:, h, :]), reads=["cand"], writes=["ctop"])
                            QQ.op("dve", lambda e, h=h: e.max_index(out=cpos[:, h, 0:8], in_max=ctop[:, h, 0:8], in_values=cand[:, h, :]),
                                 reads=["cand", "ctop"], writes=["cpos"])
                            QQ.op("dve", lambda e, h=h: e.match_replace(out=tmpc2[:], in_to_replace=ctop[:, h, 0:8],
                                                                       in_values=cand[:, h, :], imm_value=-1e30),
                                 reads=["cand", "ctop"], writes=["tmpc2"])
                            QQ.op("dve", lambda e, h=h: e.max(out=ctop[:, h, 8:16], in_=tmpc2[:]), reads=["tmpc2"], writes=["ctop"])
                            QQ.op("dve", lambda e, h=h: e.max_index(out=cpos[:, h, 8:16], in_max=ctop[:, h, 8:16], in_values=tmpc2[:]),
                                 reads=["tmpc2", "ctop"], writes=["cpos"])
                        QQ.op("dve", lambda e: e.tensor_scalar(out=au[:], in0=cpos[:], scalar1=4, scalar2=None,
                                                              op0=ALU.logical_shift_right), reads=["cpos"], writes=["au"])
                        QQ.op("dve", lambda e: e.tensor_scalar(out=bu[:], in0=cpos[:], scalar1=15, scalar2=None,
                                                              op0=ALU.bitwise_and), reads=["cpos"], writes=["bu"])
                        QQ.op("dve", lambda e: e.tensor_copy(out=af_[:], in_=au[:]), reads=["au"], writes=["af_"])
                        QQ.op("dve", lambda e: e.tensor_copy(out=bf_[:], in_=bu[:]), reads=["bu"], writes=["bf_"])
                        QQ.op("dve", lambda e: e.tensor_copy(out=itf[:], in_=itop[:]), reads=["itop"], writes=["itf"])
                        for (sel_f, c_, dst, dres) in ((af_, 0, iidx, "iidx"), (bf_, 1, jidx, "jidx")):
                            QQ.op("dve", lambda e, sel_f=sel_f: e.tensor_tensor(
                                out=eq[:], in0=sel_f[:].unsqueeze(3).to_broadcast([128, 8, 16, 16]),
                                in1=iota128[:, 0:16].unsqueeze(1).unsqueeze(1).to_broadcast([128, 8, 16, 16]), op=ALU.is_equal),
                                reads=["af_", "bf_", "iota128"], writes=["eq"])
                            QQ.op("dve", lambda e, c_=c_: e.tensor_tensor(
                                out=eq[:], in0=eq[:], in1=itf4[:, :, c_, :].unsqueeze(2).to_broadcast([128, 8, 16, 16]), op=ALU.mult),
                                reads=["eq", "itf"], writes=["eq"])
                            QQ.op("dve", lambda e, dst=dst: e.tensor_reduce(out=dst[:], in_=eq[:], axis=AX.X, op=ALU.add),
                                 reads=["eq"], writes=[dres])
                        QQ.op("dve", lambda e: e.tensor_tensor(out=ctop[:], in0=ctop[:], in1=ctop[:, :, 0:1].to_broadcast([128, 8, 16]),
                                                              op=ALU.subtract), reads=["ctop"], writes=["ctop"])
                        QQ.op("act", lambda e: e.activation(out=ctop[:], in_=ctop[:], func=ACT.Exp), reads=["ctop"], writes=["ctop"])
                        QQ.op("dve", lambda e: e.tensor_reduce(out=gz[:], in_=ctop[:], axis=AX.X, op=ALU.add), reads=["ctop"], writes=["gz"])
                        QQ.op("dve", lambda e: e.reciprocal(out=gz[:], in_=gz[:]), reads=["gz"], writes=["gz"])
                        QQ.op("dve", lambda e: e.tensor_tensor(out=ctop[:], in0=ctop[:], in1=gz[:].unsqueeze(2).to_broadcast([128, 8, 16]),
                                                              op=ALU.mult), reads=["ctop", "gz"], writes=["ctop"])
                        for (src, dstT, sres, dres) in ((iidx, iT, "iidx", "iT"), (jidx, jT, "jidx", "jT"), (ctop, gT, "ctop", "gT")):
                            b = cbank()
                            QQ.op("pe", lambda e, b=b, src=src: e.transpose(out=ps[b][:, 0:128], in_=src[:].rearrange("p h r -> p (h r)"),
                                                                           identity=identf[:]),
                                 reads=[sres, "identf"], writes=["ps%d" % b])
                            QQ.op("act", lambda e, b=b, dstT=dstT, s_=s_: e.activation(out=dstT[:, s_ * 128:(s_ + 1) * 128],
                                                                                    in_=ps[b][:, 0:128], func=ACT.Copy),
                                 reads=["ps%d" % b], writes=[dres])
                        return Q.steps

                    chains = [[], []]
                    for s_ in range(NS):
                        chains[s_ % 2].extend(tile_chain(s_, s_ % 2))
                    while any(chains):
                        for ch_ in chains:
                            replay(ch_, 1)
                    S.full_barrier()

            def gbuild_steps(tg):
                par = tg % 2
                iT, jT, gT = iTs[par], jTs[par], gTs[par]
                dsteps, psteps, asteps = [], [], []
                for t in range(TGn):
                    o = t % 8
                    Q = Rec()
                    Q.op("dve", lambda e, o=o, t=t: e.tensor_scalar(out=ohj[o][:], in0=iota128b[:], scalar1=jT[:, t:t + 1],
                                                                    scalar2=None, op0=ALU.is_equal),
                         reads=["iota128b", "jT%d_s%d" % (par, t // 128)], writes=["ohj%d" % o])
                    Q.op("dve", lambda e, o=o, t=t: e.tensor_scalar(out=ohi[o][:], in0=iota128b[:],
                                                                    scalar1=iT[:, t:t + 1], scalar2=gT[:, t:t + 1],
                                                                    op0=ALU.is_equal, op1=ALU.mult),
                         reads=["iota128b", "iT%d_s%d" % (par, t // 128), "gT%d_s%d" % (par, t // 128)], writes=["ohi%d" % o])
                    dsteps.append(Q.steps)
                    if t % 4 == 0:
                        bg = 6 + (t // 4) % 2
                    Q = Rec()
                    Q.op("pe", lambda e, bg=bg, o=o, t=t: e.matmul(out=ps[bg][:, (t % 4) * 128:(t % 4 + 1) * 128],
                                                                   lhsT=ohj[o][:], rhs=ohi[o][:], start=True, stop=True),
                         reads=["ohj%d" % o, "ohi%d" % o], writes=["ps%d" % bg])
                    psteps.append(Q.steps)
                    Q = Rec()
                    if t % 4 == 3:
                        sl = (t // TB) % 2
                        tq = (t % TB) // 4
                        Q.op("act", lambda e, bg=bg, sl=sl, tq=tq: e.activation(
                            out=gst[sl][:, :, tq * 4:(tq + 1) * 4, :].rearrange("p g t c -> p t g c"),
                            in_=ps[bg][:, :].rearrange("p (t g c) -> p t g c", t=4, g=NGR), func=ACT.Copy),
                            reads=["ps%d" % bg], writes=["gst%d" % sl])
                    if t % TB == TB - 1:
                        sl = (t // TB) % 2
                        tb0 = t - (TB - 1)
                        Q.dma("pool", out=Gd2[par][:, :, tb0:tb0 + TB, :].rearrange("g j t c -> j g (t c)"),
                              in_=gst[sl][:].rearrange("p g t c -> p g (t c)"), reads=["gst%d" % sl], writes=["Gd%d" % par])
                    asteps.append(Q.steps)
                LD, LA = 6, 4
                out = []
                for t in range(TGn + LD + LA):
                    if t < TGn:
                        out.extend(dsteps[t])
                    if 0 <= t - LD < TGn:
                        out.extend(psteps[t - LD])
                    if 0 <= t - LD - LA < TGn:
                        out.extend(asteps[t - LD - LA])
                return out

            def expert_scope(tg, nxt):
                pump_n = len(nxt) // ((NGR - 1) * (2 * CG + 2 * NS)) + 1
                par = tg % 2
                with ExitStack() as ese:
                    def sbe(name, shape, dt):
                        return ese.enter_context(nc.sbuf_tensor(uniq(name), list(shape), dt))
                    acc = sbe("acc", [128, NS, D], F32)
                    utc = [sbe("utc%d" % i, [128, KD, 128], BF16) for i in range(4)]
                    vg = [sbe("vg%d" % i, [128, CG, D], BF16) for i in range(2)]
                    gb = [sbe("gb%d" % i, [128, TGn, CG], BF16) for i in range(2)]
                    gl = [sbe("gl%d" % i, [128, TGn], F32) for i in range(2)]
                    ga = [sbe("ga%d" % i, [128, CG, TGn], BF16) for i in range(2)]
                    yt = sbe("yt", [128, D], F32)
                    pairs = ((0, 1), (2, 3))
                    pi = 0
                    for g in range(NGR):
                        gs = g % 2
                        S.dma("pool", out=gb[gs][:].rearrange("p t c -> p (t c)"), in_=Gd2[par][g].rearrange("j t c -> j (t c)"),
                              reads=["Gd%d" % par], writes=["gb%d" % gs])
                        for c in range(CG):
                            ich = g * CG + c
                            us = ich % 4
                            S.dma("sp", out=utc[us][:].rearrange("p k j -> p (k j)"), in_=UTs[ich], writes=["utc%d" % us])
                            S.dma("sp", out=vg[gs][:, c, :], in_=Vs[ich], writes=["vg%d_%d" % (gs, c)])
                            ba = 4 + ich % 2
                            for k in range(KD):
                                S.op("pe", lambda e, ba=ba, k=k, us=us: e.matmul(
                                    out=ps[ba][:, 0:TGn], lhsT=utc[us][:, k, :], rhs=xn2Ts[par][:, k, :],
                                    start=(k == 0), stop=(k == KD - 1)), reads=["utc%d" % us] + ["xn2T%d_s%d" % (par, q_) for q_ in range(NS)], writes=["ps%d" % ba], inc=(k == KD - 1))
                            replay(nxt, pump_n)
                            gi = ich % 2
                            S.op("act", lambda e, ba=ba, gi=gi: e.activation(out=gl[gi][:], in_=ps[ba][:, 0:TGn], func=ACT.Gelu),
                                 reads=["ps%d" % ba], writes=["gl%d" % gi])
                            S.op("dve", lambda e, gi=gi, gs=gs, c=c: e.tensor_tensor(
                                out=ga[gs][:, c, :], in0=gl[gi][:], in1=gb[gs][:, :, c], op=ALU.mult),
                                reads=["gl%d" % gi, "gb%d" % gs], writes=["ga%d_%d" % (gs, c)])
                            replay(nxt, pump_n)
                        for s_ in range(NS):
                            for hf in range(2):
                                pr = pairs[pi]
                                pi ^= 1
                                for c in range(CG):
                                    for n2 in range(2):
                                        c0 = hf * 1024 + n2 * 512
                                        S.op("pe", lambda e, pr=pr, n2=n2, gs=gs, c=c, s_=s_, c0=c0: e.matmul(
                                            out=ps[pr[n2]][:, :], lhsT=ga[gs][:, c, s_ * 128:(s_ + 1) * 128],
                                            rhs=vg[gs][:, c, c0:c0 + 512], start=(c == 0), stop=(c == CG - 1)),
                                            reads=["ga%d_%d" % (gs, c), "vg%d_%d" % (gs, c)], writes=["ps%d" % pr[n2]], inc=(c == CG - 1 and n2 == 1))
                                replay(nxt, pump_n)
                                for n2 in range(2):
                                    c0 = hf * 1024 + n2 * 512
                                    ares = "acc%d_%d" % (s_, hf * 2 + n2)
                                    if g == 0:
                                        S.op("act", lambda e, pr=pr, n2=n2, s_=s_, c0=c0: e.activation(
                                            out=acc[:, s_, c0:c0 + 512], in_=ps[pr[n2]][:, :], func=ACT.Copy),
                                            reads=["ps%d" % pr[n2]], writes=[ares])
                                    else:
                                        S.op("dve", lambda e, pr=pr, n2=n2, s_=s_, c0=c0: e.tensor_tensor(
                                            out=acc[:, s_, c0:c0 + 512], in0=ps[pr[n2]][:, :], in1=acc[:, s_, c0:c0 + 512], op=ALU.add),
                                            reads=["ps%d" % pr[n2], ares], writes=[ares])
                    for s_ in range(NS):
                        t0 = tg * TGn + s_ * 128
                        S.dma("sp", out=yt[:], in_=X1[t0:t0 + 128, :], writes=["yt"])
                        S.op("dve", lambda e, s_=s_: e.tensor_tensor(out=yt[:], in0=yt[:], in1=acc[:, s_, :], op=ALU.add),
                             reads=["yt"] + ["acc%d_%d" % (s_, q) for q in range(4)], writes=["yt"])
                        S.dma("pool", out=y[t0:t0 + 128, :], in_=yt[:], reads=["yt"])
                    replay(nxt, 10 ** 9)
                    S.full_barrier()

            routing_scope(0)
            replay(gbuild_steps(0), 10 ** 9)
            S.full_barrier()
            for tg in range(NTG5):
                if tg + 1 < NTG5:
                    routing_scope(tg + 1)
                    nxt = gbuild_steps(tg + 1)
                else:
                    nxt = []
                expert_scope(tg, nxt)
    S.barrier("sp")
    es.close()
    return nc


_NC_CACHE = {}


def kernel(**inputs):
    NSEQ, SEQ, NCORES = 2, 2048, 8
    if "nc" not in _NC_CACHE:
        _NC_CACHE["nc"] = build_program(NSEQ, SEQ)
    nc = _NC_CACHE["nc"]
    f = lambda a: np.ascontiguousarray(np.asarray(a, dtype=np.float32))
    shared = {}
    for k in ("norm_mix_g", "b_in", "conv_b", "q_norm_g", "k_norm_g", "sinks", "ml_norm_g", "norm_ffn_g"):
        shared[k] = f(inputs[k]).reshape(1, -1)
    for k in ("w_in", "conv_w", "w_proj_att", "w_proj_ml", "w_out", "w_peer_q", "peer_u", "peer_v"):
        shared[k] = f(inputs[k][0])
    shared["peer_keys"] = f(inputs["peer_keys"]).reshape(16, 128, 128)
    xs = f(inputs["x"]).reshape(NCORES, NSEQ * SEQ, D)
    in_maps = [dict(shared, x=xs[c]) for c in range(NCORES)]
    res = run_bass_kernel_spmd(nc, in_maps, core_ids=list(range(NCORES)))
    out = np.stack([np.asarray(r["y"]) for r in res.results], axis=0)
    return out.reshape(NCORES * NSEQ, SEQ, D).astype(np.float32)
```

```python
from contextlib import ExitStack
import numpy as np
import concourse.bass as bass
import concourse.mybir as mybir
from concourse.bass_utils import run_bass_kernel_spmd

F32 = mybir.dt.float32
BF16 = mybir.dt.bfloat16
U32 = mybir.dt.uint32
I32 = mybir.dt.int32
ACT = mybir.ActivationFunctionType
ALU = mybir.AluOpType
AX = mybir.AxisListType

D = 2048
KD = 16
INW = 9736
EPS = 1e-6
NEXP = 16384
P0_CHUNKS = 128


class Sched:
    NDS = 48

    def __init__(self, nc, es):
        self.nc = nc
        self.eng = {"pe": nc.tensor, "act": nc.scalar, "dve": nc.vector, "pool": nc.gpsimd, "sp": nc.sync}
        self.sem = {}
        for k in self.eng:
            self.sem[k] = es.enter_context(nc.semaphore("s_" + k))
        for i in range(self.NDS):
            self.sem["d%d" % i] = es.enter_context(nc.semaphore("sd%d" % i))
        self.cnt = {k: 0 for k in self.sem}
        self.seen = {k: {} for k in self.eng}
        self.dnext = 0
        self.lastw = {}
        self.readers = {}
        self.nins = 0
        self.stores = {}

    def barrier(self, e):
        for k, v in self.stores.items():
            self._wait(e, k, v)

    def full_barrier(self):
        for e in self.eng:
            for k in self.sem:
                v = self.cnt[k] * (16 if k[1:].isdigit() else 1)
                if v > 0:
                    self._wait(e, k, v)

    def _wait(self, e, key, val):
        if e == "pe" and key == "pe":
            return
        if self.seen[e].get(key, 0) >= val:
            return
        self.eng[e].wait_ge(self.sem[key], val)
        self.seen[e][key] = val

    def _deps(self, e, reads, writes):
        need = {}
        for r in reads:
            t = self.lastw.get(r)
            if t is not None:
                need[t[0]] = max(need.get(t[0], 0), t[1])
        for w in writes:
            t = self.lastw.get(w)
            if t is not None:
                need[t[0]] = max(need.get(t[0], 0), t[1])
            for k, v in self.readers.get(w, {}).items():
                need[k] = max(need.get(k, 0), v)
        for k, v in need.items():
            self._wait(e, k, v)

    def _record(self, tok, reads, writes):
        for r in reads:
            d = self.readers.setdefault(r, {})
            d[tok[0]] = max(d.get(tok[0], 0), tok[1])
        for w in writes:
            self.lastw[w] = tok
            self.readers[w] = {}

    def op(self, e, fn, reads=(), writes=(), inc=True):
        self._deps(e, reads, writes)
        ins = fn(self.eng[e])
        if inc:
            self.cnt[e] += 1
            ins.then_inc(self.sem[e], 1)
            tok = (e, self.cnt[e])
        else:
            assert e == "pe"
            tok = (e, self.cnt[e] + 1)
        self._record(tok, reads, writes)
        self.nins += 1

    def dma(self, q, out, in_, reads=(), writes=(), **kw):
        slot = self.dnext
        self.dnext = (slot + 1) % self.NDS
        key = "d%d" % slot
        if self.cnt[key] > 0:
            self._wait(q, key, self.cnt[key] * 16)
        self._deps(q, reads, writes)
        ins = self.eng[q].dma_start(out=out, in_=in_, **kw)
        self.cnt[key] += 1
        ins.then_inc(self.sem[key], 16)
        self._record((key, self.cnt[key] * 16), reads, writes)
        self.stores[key] = self.cnt[key] * 16
        self.nins += 1

    def wait_all(self, e):
        for r, t in list(self.lastw.items()):
            self._wait(e, t[0], t[1])


def build_program(NSEQ, SEQ, phases=(0, 1, 2, 3, 4, 5), debug=False):
    T = NSEQ * SEQ
    NT = T // 128
    TPS = SEQ // 128
    GT = min(512, SEQ)
    NTG = GT // 128
    NG = T // GT
    nc = bass.Bass("TRN2", target_bir_lowering=False)
    es = ExitStack()

    def din(name, shape):
        return nc.dram_tensor(name, list(shape), F32, kind="ExternalInput").ap()

    dbg_kind = "ExternalOutput" if debug else "Internal"

    def dscr(name, shape, dt):
        return nc.dram_tensor(name, list(shape), dt, kind=dbg_kind).ap()

    x = din("x", [T, D])
    norm_mix_g = din("norm_mix_g", [1, D])
    w_in = din("w_in", [D, INW])
    b_in = din("b_in", [1, INW])
    conv_w = din("conv_w", [4, 2048])
    conv_b = din("conv_b", [1, 2048])
    q_norm_g = din("q_norm_g", [1, 64])
    k_norm_g = din("k_norm_g", [1, 64])
    sinks = din("sinks", [1, 16])
    ml_norm_g = din("ml_norm_g", [1, 1024])
    w_proj_att = din("w_proj_att", [1024, D])
    w_proj_ml = din("w_proj_ml", [1024, D])
    w_out = din("w_out", [D, D])
    norm_ffn_g = din("norm_ffn_g", [1, D])
    if 0 in phases or 5 in phases:
        w_peer_q = din("w_peer_q", [D, D])
        peer_keys = din("peer_keys", [16, 128, 128])
        peer_u = din("peer_u", [NEXP, D])
        peer_v = din("peer_v", [NEXP, D])
    y = nc.dram_tensor("y", [T, D], F32, kind="ExternalOutput").ap()

    Ptm = dscr("Ptm", [T, 3584], F32)
    Pqk = dscr("Pqk", [2048, T], F32)
    Pif = dscr("Pif", [8, T], F32)
    Pg = dscr("Pg", [4096, T], F32)
    AttT = dscr("AttT", [1024, T], BF16)
    MlT = dscr("MlT", [1024, T], BF16)
    if 5 in phases and 4 not in phases:
        X1 = din("X1", [T, D])
    else:
        X1 = dscr("X1", [T, D], F32)
    MgT = dscr("MgT", [2048, T], BF16)
    UTs = dscr("UTs", [128, 128, 2048], BF16)
    Vs = dscr("Vs", [128, 128, 2048], BF16)
    WQs = dscr("WQs", [16, 128, KD, 128], BF16)

    S = Sched(nc, es)

    class Rec:
        def __init__(self):
            self.steps = []
            self.grp = None

        def begin(self):
            self.grp = []

        def end(self):
            self.steps.append(("grp", self.grp, None))
            self.grp = None

        def op(self, *a, **k):
            (self.grp if self.grp is not None else self.steps).append(("op", a, k))

        def dma(self, *a, **k):
            (self.grp if self.grp is not None else self.steps).append(("dma", a, k))

    def emit(st):
        kind, a, k = st
        if kind == "grp":
            for x in a:
                emit(x)
        else:
            (S.op if kind == "op" else S.dma)(*a, **k)

    def replay(steps, n):
        while n > 0 and steps:
            emit(steps.pop(0))
            n -= 1
    _uid = [0]

    def uniq(name):
        _uid[0] += 1
        return "%s_u%d" % (name, _uid[0])

    def sb(name, shape, dt):
        return es.enter_context(nc.sbuf_tensor(uniq(name), list(shape), dt))

    ps = [es.enter_context(nc.psum_tensor("ps%d" % i, [128, 512], F32)) for i in range(8)]

    def psb(i):
        return ps[i][:].bitcast(BF16)

    identf = sb("identf", [128, 128], F32)
    identb = sb("identb", [128, 128], BF16)
    S.op("pool", lambda e: e.memset(identf[:], 0.0), writes=["identf"])
    S.op("pool", lambda e: e.affine_select(out=identf[:], in_=identf[:], compare_op=ALU.not_equal, fill=1.0,
                                            base=0, pattern=[[-1, 128]], channel_multiplier=1),
         reads=["identf"], writes=["identf"])
    S.op("dve", lambda e: e.tensor_copy(out=identb[:], in_=identf[:]), reads=["identf"], writes=["identb"])

    def bload(dst, src_row, n, res):
        S.dma("sp", out=dst, in_=src_row.partition_broadcast(dst.shape[0]), writes=[res])


    bank = [0]

    def nbank():
        b = bank[0]
        bank[0] = (b + 1) % 8
        return b

    def p0_record(sb0, nbuf=2):
        Q = Rec()
        ust = [sb0("ust%d" % i, [128, D], F32) for i in range(nbuf)]
        vst = [sb0("vst%d" % i, [128, D], F32) for i in range(nbuf)]
        utb = [sb0("utb%d" % i, [128, KD * 128], BF16) for i in range(nbuf)]
        vbf = [sb0("vbf%d" % i, [128, D], BF16) for i in range(nbuf)]
        for ch in range(P0_CHUNKS):
            sl = ch % nbuf
            Q.dma("sp", out=ust[sl][:], in_=peer_u[ch * 128:(ch + 1) * 128, :], writes=["ust%d" % sl])
            for kq in range(4):
                b = 4 + kq % 2
                Q.begin()
                for j in range(4):
                    k = kq * 4 + j
                    Q.op("pe", lambda e, b=b, j=j, k=k, sl=sl: e.transpose(
                        out=ps[b][:, j * 128:(j + 1) * 128], in_=ust[sl][:, k * 128:(k + 1) * 128], identity=identf[:]),
                        reads=["ust%d" % sl, "identf"], writes=["ps%d" % b])
                Q.op("act" if kq % 2 == 0 else "dve", lambda e, b=b, kq=kq, sl=sl: (
                    e.activation(out=utb[sl][:, kq * 512:(kq + 1) * 512], in_=ps[b][:, :], func=ACT.Copy)
                    if kq % 2 == 0 else e.tensor_copy(out=utb[sl][:, kq * 512:(kq + 1) * 512], in_=ps[b][:, :])),
                    reads=["ps%d" % b], writes=["utb%d_%d" % (sl, kq)])
                Q.end()
            Q.dma("pool", out=UTs[ch], in_=utb[sl][:], reads=["utb%d_%d" % (sl, kq) for kq in range(4)])
            Q.dma("sp", out=vst[sl][:], in_=peer_v[ch * 128:(ch + 1) * 128, :], writes=["vst%d" % sl])
            Q.op("act", lambda e, sl=sl: e.activation(out=vbf[sl][:, 0:1024], in_=vst[sl][:, 0:1024], func=ACT.Copy),
                 reads=["vst%d" % sl], writes=["vbf%da" % sl])
            Q.op("dve", lambda e, sl=sl: e.tensor_copy(out=vbf[sl][:, 1024:2048], in_=vst[sl][:, 1024:2048]),
                 reads=["vst%d" % sl], writes=["vbf%db" % sl])
            Q.dma("pool", out=Vs[ch], in_=vbf[sl][:], reads=["vbf%da" % sl, "vbf%db" % sl])
        for k in range(KD):
            sl = k % nbuf
            Q.dma("sp", out=vst[sl][:], in_=w_peer_q[k * 128:(k + 1) * 128, :], writes=["vst%d" % sl])
            Q.op("act", lambda e, sl=sl: e.activation(out=vbf[sl][:, 0:1024], in_=vst[sl][:, 0:1024], func=ACT.Copy),
                 reads=["vst%d" % sl], writes=["vbf%da" % sl])
            Q.op("dve", lambda e, sl=sl: e.tensor_copy(out=vbf[sl][:, 1024:2048], in_=vst[sl][:, 1024:2048]),
                 reads=["vst%d" % sl], writes=["vbf%db" % sl])
            Q.dma("sp", out=WQs[:, :, k, :].rearrange("b p c -> p b c"), in_=vbf[sl][:].rearrange("p (b c) -> p b c", c=128),
                  reads=["vbf%da" % sl, "vbf%db" % sl])
        return Q.steps

    p0_in_p3 = (0 in phases) and (3 in phases)
    if 0 in phases and not p0_in_p3:
        with ExitStack() as es0:
            def sb0(name, shape, dt):
                return es0.enter_context(nc.sbuf_tensor(uniq(name), list(shape), dt))
            replay(p0_record(sb0), 10 ** 9)
            S.full_barrier()

    if 1 in phases:
        GT1 = min(1024, SEQ)
        NTG1 = GT1 // 128
        NG1 = T // GT1
        HW = min(512, GT1)
        HN = GT1 // HW
        with ExitStack() as es1:
            def sb1(name, shape, dt):
                return es1.enter_context(nc.sbuf_tensor(uniq(name), list(shape), dt))
            gmix = sb1("gmix", [128, D], F32)
            bload(gmix[:], norm_mix_g[0:1, :], D, "gmix")
            bias_tm = sb1("bias_tm", [128, 3584], F32)
            S.dma("sp", out=bias_tm[:, 0:1536], in_=b_in[0:1, 0:1536].partition_broadcast(128), writes=["bias_tm"])
            S.dma("sp", out=bias_tm[:, 1536:3584], in_=b_in[0:1, 3584:5632].partition_broadcast(128), writes=["bias_tm"])
            bcol_qk = sb1("bcol_qk", [128, 16], F32)
            bcol_g = sb1("bcol_g", [128, 32], F32)
            bcol_i = sb1("bcol_i", [4, 1], F32)
            bcol_f = sb1("bcol_f", [4, 1], F32)
            S.dma("sp", out=bcol_qk[:], in_=b_in[0, 1536:3584].rearrange("(b p) -> p b", p=128),
                  writes=["bcol_qk"], allow_slow_non_contiguous=True)
            S.dma("sp", out=bcol_g[:], in_=b_in[0, 5640:9736].rearrange("(b p) -> p b", p=128),
                  writes=["bcol_g"], allow_slow_non_contiguous=True)
            S.dma("sp", out=bcol_i[:], in_=b_in[0, 5632:5636].rearrange("(p b) -> p b", b=1),
                  writes=["bcol_i"], allow_slow_non_contiguous=True)
            S.dma("sp", out=bcol_f[:], in_=b_in[0, 5636:5640].rearrange("(p b) -> p b", b=1),
                  writes=["bcol_f"], allow_slow_non_contiguous=True)

            xt = sb1("xt", [128, D], F32)
            junk = sb1("junk", [128, D], BF16)
            ssq = sb1("ssq", [128, 1], F32)
            rstd = sb1("rstd", [128, 1], F32)
            xnb = sb1("xnb", [128, D], BF16)
            xnT = sb1("xnT", [128, KD, GT1], BF16)
            wsts = [sb1("wst%d" % i, [128, KD, 512], F32) for i in range(2)]
            wbf = [sb1("wbf%d" % i, [128, KD, 512], BF16) for i in range(2)]
            stg = [sb1("stg%d" % i, [128, 512], F32) for i in range(3)]
            stgif = sb1("stgif", [4, HW], F32)
            w_in_v = w_in.rearrange("(k p) n -> p k n", p=128)

            chunks = []
            for c in range(3):
                chunks.append((c * 512, 512, "tm", c * 512))
            for c in range(4):
                chunks.append((1536 + c * 512, 512, "qk", c * 4))
            for c in range(4):
                chunks.append((3584 + c * 512, 512, "tm", 1536 + c * 512))
            chunks.append((5632, 8, "if", 0))
            for c in range(8):
                chunks.append((5640 + c * 512, 512, "g", c * 4))

            bank = [0]
            stgi = [0]

            def nbank():
                b = bank[0]
                bank[0] = (b + 1) % 8
                return b

            def nstg():
                i = stgi[0]
                stgi[0] = (i + 1) % 3
                return i

            for g in range(NG1):
                for ti in range(NTG1):
                    t0 = g * GT1 + ti * 128
                    S.dma("sp", out=xt[:], in_=x[t0:t0 + 128, :], writes=["xt"])
                    S.op("act", lambda e: e.activation(out=junk[:], in_=xt[:], func=ACT.Square, accum_out=ssq[:]),
                         reads=["xt"], writes=["junk", "ssq"])
                    S.op("dve", lambda e: e.tensor_scalar(out=rstd[:], in0=ssq[:], scalar1=1.0 / D, scalar2=EPS,
                                                          op0=ALU.mult, op1=ALU.add), reads=["ssq"], writes=["rstd"])
                    S.op("act", lambda e: e.activation(out=rstd[:], in_=rstd[:], func=ACT.Sqrt),
                         reads=["rstd"], writes=["rstd"])
                    S.op("dve", lambda e: e.reciprocal(out=rstd[:], in_=rstd[:]), reads=["rstd"], writes=["rstd"])
                    S.op("dve", lambda e: e.scalar_tensor_tensor(out=xnb[:], in0=xt[:], scalar=rstd[:, 0:1], in1=gmix[:],
                                                                 op0=ALU.mult, op1=ALU.mult),
                         reads=["xt", "rstd", "gmix"], writes=["xnb"])
                    for kq in range(4):
                        b = nbank()
                        for j in range(4):
                            k = kq * 4 + j
                            S.op("pe", lambda e, k=k, j=j, b=b: e.transpose(out=psb(b)[:, j * 128:(j + 1) * 128],
                                                                            in_=xnb[:, k * 128:(k + 1) * 128],
                                                                            identity=identb[:]),
                                 reads=["xnb", "identb"], writes=["ps%d" % b])
                        S.op("act", lambda e, kq=kq, b=b, ti=ti: e.activation(
                            out=xnT[:, kq * 4:(kq + 1) * 4, ti * 128:(ti + 1) * 128],
                            in_=psb(b)[:, 0:512].rearrange("p (j t) -> p j t", j=4), func=ACT.Copy),
                            reads=["ps%d" % b], writes=["xnT"])
                for ci, (c0, ncol, kind, aux) in enumerate(chunks):
                    wb = wbf[ci % 2]
                    wres = "wbf%d" % (ci % 2)
                    wst = wsts[ci % 2]
                    wstres = "wst%d" % (ci % 2)
                    S.dma("sp", out=wst[:, :, 0:ncol], in_=w_in_v[:, :, c0:c0 + ncol], writes=[wstres])
                    S.op("dve", lambda e, wb=wb, ncol=ncol, wst=wst: e.tensor_copy(out=wb[:, 0:8, 0:ncol], in_=wst[:, 0:8, 0:ncol]),
                         reads=[wstres], writes=[wres + "a"])
                    S.op("act", lambda e, wb=wb, ncol=ncol, wst=wst: e.activation(out=wb[:, 8:16, 0:ncol], in_=wst[:, 8:16, 0:ncol], func=ACT.Copy),
                         reads=[wstres], writes=[wres + "b"])
                    wr = [wres + "a", wres + "b"]
                    if kind == "tm":
                        for ti in range(NTG1):
                            t0 = g * GT1 + ti * 128
                            b = nbank()
                            for k in range(KD):
                                S.op("pe", lambda e, k=k, b=b, ti=ti, wb=wb: e.matmul(
                                    out=ps[b][:, :], lhsT=xnT[:, k, ti * 128:(ti + 1) * 128], rhs=wb[:, k, :],
                                    start=(k == 0), stop=(k == KD - 1)),
                                    reads=["xnT"] + wr, writes=["ps%d" % b], inc=(k == KD - 1))
                            si = nstg()
                            S.op("dve", lambda e, b=b, si=si, aux=aux: e.tensor_tensor(
                                out=stg[si][:], in0=ps[b][:, :], in1=bias_tm[:, aux:aux + 512], op=ALU.add),
                                reads=["ps%d" % b, "bias_tm"], writes=["stg%d" % si])
                            S.dma("pool", out=Ptm[t0:t0 + 128, aux:aux + 512], in_=stg[si][:], reads=["stg%d" % si])
                    elif kind in ("qk", "g"):
                        for bl in range(4):
                          for hv in range(HN):
                            b = nbank()
                            for k in range(KD):
                                S.op("pe", lambda e, k=k, b=b, bl=bl, wb=wb, hv=hv: e.matmul(
                                    out=ps[b][:, 0:HW], lhsT=wb[:, k, bl * 128:(bl + 1) * 128], rhs=xnT[:, k, hv * HW:(hv + 1) * HW],
                                    start=(k == 0), stop=(k == KD - 1)),
                                    reads=["xnT"] + wr, writes=["ps%d" % b], inc=(k == KD - 1))
                            si = nstg()
                            blk = aux + bl
                            c_lo = g * GT1 + hv * HW
                            if kind == "qk":
                                S.op("act", lambda e, b=b, si=si, blk=blk: e.activation(
                                    out=stg[si][:, 0:HW], in_=ps[b][:, 0:HW], func=ACT.Identity,
                                    bias=bcol_qk[:, blk:blk + 1], scale=1.0),
                                    reads=["ps%d" % b, "bcol_qk"], writes=["stg%d" % si])
                                S.dma("pool", out=Pqk[blk * 128:(blk + 1) * 128, c_lo:c_lo + HW],
                                      in_=stg[si][:, 0:HW], reads=["stg%d" % si])
                            else:
                                S.op("act", lambda e, b=b, si=si, blk=blk: e.activation(
                                    out=stg[si][:, 0:HW], in_=ps[b][:, 0:HW], func=ACT.Sigmoid,
                                    bias=bcol_g[:, blk:blk + 1], scale=1.0),
                                    reads=["ps%d" % b, "bcol_g"], writes=["stg%d" % si])
                                S.dma("pool", out=Pg[blk * 128:(blk + 1) * 128, c_lo:c_lo + HW],
                                      in_=stg[si][:, 0:HW], reads=["stg%d" % si])
                    else:
                        for half, bc in ((0, bcol_i), (1, bcol_f)):
                          for hv in range(HN):
                            b = nbank()
                            for k in range(KD):
                                S.op("pe", lambda e, k=k, b=b, half=half, wb=wb, hv=hv: e.matmul(
                                    out=ps[b][0:4, 0:HW], lhsT=wb[:, k, half * 4:half * 4 + 4], rhs=xnT[:, k, hv * HW:(hv + 1) * HW],
                                    start=(k == 0), stop=(k == KD - 1)),
                                    reads=["xnT"] + wr, writes=["ps%d" % b], inc=(k == KD - 1))
                            S.op("act", lambda e, b=b, bc=bc: e.activation(
                                out=stgif[:, :], in_=ps[b][0:4, 0:HW], func=ACT.Identity, bias=bc[:, 0:1], scale=1.0),
                                reads=["ps%d" % b, "bcol_i", "bcol_f"], writes=["stgif"])
                            c_lo = g * GT1 + hv * HW
                            S.dma("pool", out=Pif[half * 4:half * 4 + 4, c_lo:c_lo + HW], in_=stgif[:, :],
                                  reads=["stgif"])
            S.full_barrier()

    bank = [0]

    def nbank():
        b = bank[0]
        bank[0] = (b + 1) % 8
        return b

    def p2_record(sb2):
        Q = Rec()
        gq = sb2("gq", [128, 64], F32)
        gk = sb2("gk", [128, 64], F32)
        Q.dma("sp", out=gq[:], in_=q_norm_g[0:1, :].partition_broadcast(128), writes=["gq"])
        Q.dma("sp", out=gk[:], in_=k_norm_g[0:1, :].partition_broadcast(128), writes=["gk"])
        Q.op("dve", lambda e: e.tensor_scalar(out=gq[:], in0=gq[:], scalar1=0.125, scalar2=None, op0=ALU.mult),
             reads=["gq"], writes=["gq"])
        esk = sb2("esk", [64, 16], F32)
        Q.dma("sp", out=esk[:], in_=sinks[0:1, :].partition_broadcast(64), writes=["esk"])
        Q.op("act", lambda e: e.activation(out=esk[:], in_=esk[:], func=ACT.Exp), reads=["esk"], writes=["esk"])
        maskc = sb2("maskc", [128, 128], F32)
        maskp = sb2("maskp", [128, 128], F32)
        Q.op("pool", lambda e: e.memset(maskc[:], 1.0), writes=["maskc"])
        Q.op("pool", lambda e: e.affine_select(out=maskc[:], in_=maskc[:], compare_op=ALU.is_ge, fill=0.0,
                                                base=0, pattern=[[1, 128]], channel_multiplier=-1),
             reads=["maskc"], writes=["maskc"])
        Q.op("dve", lambda e: e.tensor_scalar(out=maskp[:], in0=maskc[:], scalar1=-1.0, scalar2=1.0,
                                              op0=ALU.mult, op1=ALU.add), reads=["maskc"], writes=["maskp"])
        ones_bf = sb2("ones_bf", [128, 64], BF16)
        Q.op("pool", lambda e: e.memset(ones_bf[:], 1.0), writes=["ones_bf"])
        qt = sb2("qt", [128, 1024], F32)
        kt = sb2("kt", [128, 256], F32)
        vt = sb2("vt", [128, 256], F32)
        sq = sb2("sq", [128, 1024], F32)
        ssq16 = sb2("ssq16", [128, 16], F32)
        ssq4 = sb2("ssq4", [128, 4], F32)
        qn = sb2("qn", [128, 1024], BF16)
        kn = sb2("kn", [128, 256], BF16)
        qT = sb2("qT", [64, 16, 128], BF16)
        kT = [sb2("kT%d" % i, [64, 4, 128], BF16) for i in range(2)]
        vb = [sb2("vb%d" % i, [128, 256], BF16) for i in range(2)]
        E = sb2("E", [128, 512], F32)
        PTc = sb2("PTc", [128, 512], BF16)
        PTp = sb2("PTp", [128, 512], BF16)
        den = sb2("den", [64, 512], F32)
        attT = sb2("attT", [64, 16, 128], BF16)
        AttT_v = AttT.rearrange("(h d) t -> d h t", d=64)

        def headnorm(src, nh, ssqt, gt, dst, sres, dres):
            w = nh * 64
            Q.op("dve", lambda e: e.tensor_tensor(out=sq[:, 0:w], in0=src[:, 0:w], in1=src[:, 0:w], op=ALU.mult),
                 reads=[sres], writes=["sq"])
            Q.op("dve", lambda e: e.tensor_reduce(out=ssqt[:], in_=sq[:, 0:w].rearrange("p (h d) -> p h d", d=64),
                                                  axis=AX.X, op=ALU.add), reads=["sq"], writes=["ssqt"])
            Q.op("dve", lambda e: e.tensor_scalar(out=ssqt[:], in0=ssqt[:], scalar1=1.0 / 64, scalar2=EPS,
                                                  op0=ALU.mult, op1=ALU.add), reads=["ssqt"], writes=["ssqt"])
            Q.op("act", lambda e: e.activation(out=ssqt[:], in_=ssqt[:], func=ACT.Sqrt), reads=["ssqt"], writes=["ssqt"])
            Q.op("dve", lambda e: e.reciprocal(out=ssqt[:], in_=ssqt[:]), reads=["ssqt"], writes=["ssqt"])
            Q.op("dve", lambda e: e.tensor_tensor(out=sq[:, 0:w].rearrange("p (h d) -> p h d", d=64),
                                                  in0=src[:, 0:w].rearrange("p (h d) -> p h d", d=64),
                                                  in1=ssqt[:].unsqueeze(2).to_broadcast([128, nh, 64]), op=ALU.mult),
                 reads=[sres, "ssqt"], writes=["sq"])
            Q.op("dve", lambda e: e.tensor_tensor(out=dst[:, 0:w].rearrange("p (h d) -> p h d", d=64),
                                                  in0=sq[:, 0:w].rearrange("p (h d) -> p h d", d=64),
                                                  in1=gt[:].unsqueeze(1).to_broadcast([128, nh, 64]), op=ALU.mult),
                 reads=["sq", "gq", "gk"], writes=[dres])

        for s_ in range(NSEQ):
            for qb in range(TPS):
                t0 = s_ * SEQ + qb * 128
                cur = qb % 2
                prv = 1 - cur
                Q.dma("sp", out=qt[:], in_=Ptm[t0:t0 + 128, 0:1024], writes=["qt"])
                Q.dma("sp", out=kt[:], in_=Ptm[t0:t0 + 128, 1024:1280], writes=["kt"])
                Q.dma("sp", out=vt[:], in_=Ptm[t0:t0 + 128, 1280:1536], writes=["vt"])
                headnorm(qt, 16, ssq16, gq, qn, "qt", "qn")
                headnorm(kt, 4, ssq4, gk, kn, "kt", "kn")
                Q.op("act", lambda e, cur=cur: e.activation(out=vb[cur][:], in_=vt[:], func=ACT.Copy),
                     reads=["vt"], writes=["vb%d" % cur])
                for half in range(2):
                    b = 6 + half
                    Q.begin()
                    for h8 in range(8):
                        h = half * 8 + h8
                        Q.op("pe", lambda e, b=b, h=h, h8=h8: e.transpose(
                            out=psb(b)[0:64, h8 * 128:(h8 + 1) * 128], in_=qn[:, h * 64:(h + 1) * 64], identity=identb[:]),
                            reads=["qn", "identb"], writes=["ps%d" % b])
                    Q.end()
                    Q.op("act", lambda e, b=b, half=half: e.activation(
                        out=qT[:, half * 8:(half + 1) * 8, :],
                        in_=psb(b)[0:64, :].rearrange("p (h t) -> p h t", h=8), func=ACT.Copy),
                        reads=["ps%d" % b], writes=["qT"])
                b = 6
                Q.begin()
                for h in range(4):
                    Q.op("pe", lambda e, b=b, h=h: e.transpose(
                        out=psb(b)[0:64, h * 128:(h + 1) * 128], in_=kn[:, h * 64:(h + 1) * 64], identity=identb[:]),
                        reads=["kn", "identb"], writes=["ps%d" % b])
                Q.end()
                Q.op("act", lambda e, b=b, cur=cur: e.activation(
                    out=kT[cur][:, :, :], in_=psb(b)[0:64, 0:512].rearrange("p (h t) -> p h t", h=4), func=ACT.Copy),
                    reads=["ps%d" % b], writes=["kT%d" % cur])
                for hk in range(4):
                    qTv = qT[:, 4 * hk:4 * hk + 4, :].rearrange("p h t -> p (h t)")
                    srcs = [(cur, maskc, PTc, "PTc")]
                    if qb > 0:
                        srcs.append((prv, maskp, PTp, "PTp"))
                    for (sl, mk, PT, pres) in srcs:
                        bS = 6
                        Q.op("pe", lambda e, bS=bS, sl=sl, hk=hk, qTv=qTv: e.matmul(
                            out=ps[bS][:, :], lhsT=kT[sl][:, hk, :], rhs=qTv, start=True, stop=True),
                            reads=["kT%d" % sl, "qT"], writes=["ps%d" % bS])
                        Q.op("act", lambda e, bS=bS: e.activation(out=E[:], in_=ps[bS][:, :], func=ACT.Exp),
                             reads=["ps%d" % bS], writes=["E"])
                        Q.op("dve", lambda e, mk=mk, PT=PT: e.tensor_tensor(
                            out=PT[:].rearrange("p (h t) -> p h t", h=4), in0=E[:].rearrange("p (h t) -> p h t", h=4),
                            in1=mk[:].unsqueeze(1).to_broadcast([128, 4, 128]), op=ALU.mult),
                            reads=["E", "maskc", "maskp"], writes=[pres])
                    bO = 7
                    bD = 6
                    Q.begin()
                    for i, (sl, mk, PT, pres) in enumerate(srcs):
                        Q.op("pe", lambda e, bO=bO, sl=sl, PT=PT, i=i, hk=hk: e.matmul(
                            out=ps[bO][0:64, :], lhsT=vb[sl][:, hk * 64:(hk + 1) * 64], rhs=PT[:],
                            start=(i == 0), stop=(i == len(srcs) - 1)),
                            reads=["vb%d" % sl, pres], writes=["ps%d" % bO])
                    for i, (sl, mk, PT, pres) in enumerate(srcs):
                        Q.op("pe", lambda e, bD=bD, PT=PT, i=i: e.matmul(
                            out=ps[bD][0:64, :], lhsT=ones_bf[:, 0:64], rhs=PT[:],
                            start=(i == 0), stop=(i == len(srcs) - 1)),
                            reads=["ones_bf", pres], writes=["ps%d" % bD])
                    Q.end()
                    Q.op("dve", lambda e, bD=bD, hk=hk: e.tensor_tensor(
                        out=den[:].rearrange("p (h t) -> p h t", h=4),
                        in0=ps[bD][0:64, :].rearrange("p (h t) -> p h t", h=4),
                        in1=esk[:, 4 * hk:4 * hk + 4].unsqueeze(2).to_broadcast([64, 4, 128]), op=ALU.add),
                        reads=["ps%d" % bD, "esk"], writes=["den"])
                    Q.op("dve", lambda e: e.reciprocal(out=den[:], in_=den[:]), reads=["den"], writes=["den"])
                    Q.op("dve", lambda e, bO=bO, hk=hk: e.tensor_tensor(
                        out=attT[:, 4 * hk:4 * hk + 4, :].rearrange("p h t -> p (h t)"),
                        in0=ps[bO][0:64, :], in1=den[:], op=ALU.mult),
                        reads=["ps%d" % bO, "den"], writes=["attT"])
                Q.dma("pool", out=AttT_v[:, :, t0:t0 + 128], in_=attT[:], reads=["attT"])
        return Q.steps

    p2_in_p3 = (2 in phases) and (3 in phases)
    if 2 in phases and not p2_in_p3:
        with ExitStack() as es2:
            def sb2(name, shape, dt):
                return es2.enter_context(nc.sbuf_tensor(uniq(name), list(shape), dt))
            replay(p2_record(sb2), 10 ** 9)
            S.full_barrier()

    if 3 in phases:
        with ExitStack() as es3:
            def sb3(name, shape, dt):
                return es3.enter_context(nc.sbuf_tensor(uniq(name), list(shape), dt))
            i4 = sb3("i4", [4, SEQ], F32)
            f4 = sb3("f4", [4, SEQ], F32)
            B4 = sb3("B4", [4, SEQ], F32)
            G4 = sb3("G4", [4, SEQ], F32)
            ones4 = sb3("ones4", [4, SEQ], F32)
            S.op("pool", lambda e: e.memset(ones4[:], 1.0), writes=["ones4"])
            sel = sb3("sel", [4, 4, 128], F32)
            S.op("pool", lambda e: e.memset(sel[:], 1.0), writes=["sel"])
            S.op("pool", lambda e: e.affine_select(out=sel[:], in_=sel[:], compare_op=ALU.is_equal, fill=0.0, base=0,
                                                    pattern=[[-1, 4], [0, 128]], channel_multiplier=1),
                 reads=["sel"], writes=["sel"])
            maskbd = sb3("maskbd", [128, 128], F32)
            S.op("pool", lambda e: e.memset(maskbd[:], 1.0), writes=["maskbd"])
            S.op("pool", lambda e: e.affine_select(out=maskbd[:], in_=maskbd[:], compare_op=ALU.is_ge, fill=0.0,
                                                    base=0, pattern=[[1, 128]], channel_multiplier=-1),
                 reads=["maskbd"], writes=["maskbd"])
            S.op("pool", lambda e: e.memset(maskbd[0:64, 64:128], 0.0), reads=["maskbd"], writes=["maskbd"])
            cw = sb3("cw", [128, 16, 4], F32)
            cb = sb3("cb", [128, 16], F32)
            for j in range(4):
                S.dma("sp", out=cw[:, :, j], in_=conv_w[j, :].rearrange("(b p) -> p b", p=128), writes=["cw"],
                      allow_slow_non_contiguous=True)
            S.dma("sp", out=cb[:], in_=conv_b[0, :].rearrange("(b p) -> p b", p=128), writes=["cb"],
                  allow_slow_non_contiguous=True)
            gml = sb3("gml", [128, 1024], F32)
            bload(gml[:], ml_norm_g[0:1, :], 1024, "gml")
            xin = sb3("xin", [128, 16, 131], F32)
            acc = sb3("acc", [128, 16, 128], F32)
            tmpc = sb3("tmpc", [128, 16, 128], F32)
            qTb = sb3("qTb", [128, 8, 128], BF16)
            kTb = sb3("kTb", [128, 8, 128], BF16)
            ktm = sb3("ktm", [128, 1024], BF16)
            vt3 = sb3("vt3", [128, 1024], F32)
            vaug = sb3("vaug", [128, 4, 257], BF16)
            S.op("pool", lambda e: e.memset(vaug[:], 1.0), writes=["vaug"])
            so = sb3("so", [128, 1024], F32)
            cols = sb3("cols", [128, 12], F32)
            emt = sb3("emt", [128, 4], F32)
            mprev = sb3("mprev", [128, 4], F32)
            argt = [sb3("argt%d" % i, [128, 128], F32) for i in range(4)]
            sctf = [sb3("sctf%d" % i, [128, 128], F32) for i in range(4)]
            sctb = [sb3("sctb%d" % i, [128, 128], BF16) for i in range(4)]
            wcol = [sb3("wcol%d" % i, [128, 1], F32) for i in range(4)]
            kw = [sb3("kw%d" % i, [128, 256], BF16) for i in range(4)]
            wbt = [sb3("wbt%d" % i, [128, 128], F32) for i in range(4)]
            qst = [sb3("qst%d" % i, [128, 2, 128], BF16) for i in range(4)]
            gendl = [sb3("gend%d" % i, [128, 2], F32) for i in range(4)]
            Cf = sb3("Cf", [128, 4, 2, 257], F32)
            Cb = sb3("Cb", [128, 4, 2, 257], BF16)
            dn = [sb3("dn%d" % i, [128, 1], F32) for i in range(4)]
            hh = [sb3("hh%d" % i, [128, 256], F32) for i in range(4)]
            junk3 = [sb3("junk3%d" % i, [128, 256], BF16) for i in range(4)]
            ssqh = [sb3("ssqh%d" % i, [128, 1], F32) for i in range(4)]
            hn = [sb3("hn%d" % i, [128, 256], F32) for i in range(4)]
            ml = sb3("ml", [128, 1024], BF16)
            mlT = sb3("mlT", [128, 8, 128], BF16)
            Pqk_v = Pqk.rearrange("(b p) t -> p b t", p=128)
            MlT_v = MlT.rearrange("(b p) t -> p b t", p=128)
            lb = [0]

            def lbank():
                lb[0] = (lb[0] + 1) % 4
                return lb[0]

            p0s = p0_record(sb3, nbuf=1) if p0_in_p3 else []
            p0_per_round = len(p0s) // (NT * 2 * 42) + 1
            p2s = p2_record(sb3) if p2_in_p3 else []
            p2_per_round = len(p2s) // (NT * 2 * 42) + 1

            for s_ in range(NSEQ):
                ts0 = s_ * SEQ
                S.dma("sp", out=i4[:], in_=Pif[0:4, ts0:ts0 + SEQ], writes=["i4"])
                S.dma("sp", out=f4[:], in_=Pif[4:8, ts0:ts0 + SEQ], writes=["f4"])
                S.op("act", lambda e: e.activation(out=f4[:], in_=f4[:], func=ACT.Exp, scale=-1.0), reads=["f4"], writes=["f4"])
                S.op("act", lambda e: e.activation(out=f4[:], in_=f4[:], func=ACT.Ln, bias=1.0), reads=["f4"], writes=["f4"])
                S.op("dve", lambda e: e.tensor_tensor_scan(out=B4[:], data0=ones4[:], data1=f4[:], initial=0.0,
                                                           op0=ALU.mult, op1=ALU.subtract),
                     reads=["ones4", "f4"], writes=["B4"])
                S.op("dve", lambda e: e.tensor_tensor(out=i4[:], in0=i4[:], in1=B4[:], op=ALU.subtract),
                     reads=["i4", "B4"], writes=["i4"])
                S.op("dve", lambda e: e.tensor_tensor_scan(out=G4[:], data0=i4[:], data1=i4[:], initial=-1e30,
                                                           op0=ALU.max, op1=ALU.max), reads=["i4"], writes=["G4"])
                S.op("pool", lambda e: e.memset(Cf[:], 0.0), writes=["Cf%d%d" % (a_, b_) for a_ in range(4) for b_ in range(2)])
                S.op("pool", lambda e: e.memset(Cb[:], 0.0), writes=["Cb%d%d" % (a_, b_) for a_ in range(4) for b_ in range(2)])
                S.op("pool", lambda e: e.memset(mprev[:], -1e30), writes=["mprev%d" % a_ for a_ in range(4)])
                for tb in range(TPS):
                    t0 = ts0 + tb * 128
                    tl = tb * 128
                    b = lbank()
                    for qi, (src, sres) in enumerate(((i4, "i4"), (G4, "G4"), (B4, "B4"))):
                        S.op("pe", lambda e, b=b, qi=qi, src=src: e.transpose(
                            out=ps[b][:, qi * 4:qi * 4 + 4], in_=src[0:4, tl:tl + 128], identity=identf[0:4, 0:4]),
                            reads=[sres, "identf"], writes=["ps%d" % b])
                    S.op("dve", lambda e, b=b: e.tensor_copy(out=cols[:], in_=ps[b][:, 0:12]), reads=["ps%d" % b], writes=["cols"])
                    S.op("dve", lambda e: e.tensor_tensor(out=emt[:], in0=cols[:, 4:8], in1=cols[:, 8:12], op=ALU.add),
                         reads=["cols"], writes=["emt"])
                    S.op("act", lambda e: e.activation(out=emt[:], in_=emt[:], func=ACT.Exp, scale=-1.0), reads=["emt"], writes=["emt"])
                    if tb == 0:
                        S.op("pool", lambda e: e.memset(xin[:, :, 0:3], 0.0), reads=["xin"], writes=["xin"])
                    else:
                        S.op("dve", lambda e: e.tensor_copy(out=xin[:, :, 0:3], in_=xin[:, :, 128:131]), reads=["xin"], writes=["xin"])
                    S.dma("sp", out=xin[:, :, 3:131], in_=Pqk_v[:, :, t0:t0 + 128], reads=["xin"], writes=["xin"])
                    S.op("dve", lambda e: e.tensor_tensor(out=acc[:], in0=xin[:, :, 0:128],
                                                          in1=cw[:, :, 0:1].to_broadcast([128, 16, 128]), op=ALU.mult),
                         reads=["xin", "cw"], writes=["acc"])
                    for j in range(1, 4):
                        S.op("dve", lambda e, j=j: e.tensor_tensor(out=tmpc[:], in0=xin[:, :, j:j + 128],
                                                                   in1=cw[:, :, j:j + 1].to_broadcast([128, 16, 128]), op=ALU.mult),
                             reads=["xin", "cw"], writes=["tmpc"])
                        S.op("dve", lambda e: e.tensor_tensor(out=acc[:], in0=acc[:], in1=tmpc[:], op=ALU.add),
                             reads=["acc", "tmpc"], writes=["acc"])
                    S.op("dve", lambda e: e.tensor_tensor(out=acc[:], in0=acc[:],
                                                          in1=cb[:].unsqueeze(2).to_broadcast([128, 16, 128]), op=ALU.add),
                         reads=["acc", "cb"], writes=["acc"])
                    S.op("act", lambda e: e.activation(out=acc[:], in_=acc[:], func=ACT.Silu), reads=["acc"], writes=["acc"])
                    S.op("dve", lambda e: e.tensor_copy(out=qTb[:], in_=acc[:, 0:8, :]), reads=["acc"], writes=["qTb"])
                    S.op("dve", lambda e: e.tensor_scalar(out=kTb[:], in0=acc[:, 8:16, :], scalar1=1.0 / 16, scalar2=None,
                                                          op0=ALU.mult), reads=["acc"], writes=["kTb"])
                    b = lbank()
                    for blk in range(8):
                        S.op("pe", lambda e, b=b, blk=blk: e.transpose(
                            out=psb(b)[:, blk * 128:(blk + 1) * 128], in_=kTb[:, blk, :], identity=identb[:]),
                            reads=["kTb", "identb"], writes=["ps%d" % b])
                    S.op("act", lambda e, b=b: e.activation(out=ktm[:], in_=psb(b)[:, :], func=ACT.Copy),
                         reads=["ps%d" % b], writes=["ktm"])
                    S.dma("sp", out=vt3[:], in_=Ptm[t0:t0 + 128, 1536:2560], writes=["vt3"])
                    S.op("act", lambda e: e.activation(out=vaug[:, :, 0:256], in_=vt3[:].rearrange("p (h d) -> p h d", h=4),
                                                       func=ACT.Copy), reads=["vt3"], writes=["vaug"])
                    S.dma("sp", out=so[:], in_=Ptm[t0:t0 + 128, 2560:3584], writes=["so"])
                    S.op("act", lambda e: e.activation(out=so[:], in_=so[:], func=ACT.Sigmoid), reads=["so"], writes=["so"])
                    def head_chain(h):
                        Q = Rec()
                        H = str(h)
                        bX = h % 2
                        bG = bX
                        Q.op("pe", lambda e: e.matmul(out=ps[bG][:, 0:128], lhsT=sel[0:4, h, :],
                                                      rhs=G4[0:4, tl:tl + 128], start=True, stop=True),
                             reads=["sel", "G4"], writes=["ps%d" % bG])
                        bS = bX
                        Q.begin()
                        for kc in range(2):
                            Q.op("pe", lambda e, kc=kc: e.matmul(
                                out=ps[bS][:, 128:256], lhsT=kTb[:, h * 2 + kc, :], rhs=qTb[:, h * 2 + kc, :],
                                start=(kc == 0), stop=(kc == 1)), reads=["kTb", "qTb"], writes=["ps%d" % bS])
                        Q.end()
                        Q.op("dve", lambda e: e.tensor_scalar(
                            out=argt[h][:], in0=ps[bG][:, 0:128], scalar1=cols[:, h:h + 1], scalar2=0.0,
                            op0=ALU.subtract, op1=ALU.max), reads=["ps%d" % bG, "cols"], writes=["argt" + H])
                        Q.op("dve", lambda e: e.tensor_copy(out=gendl[h][:, 0:1], in_=ps[bG][:, 63:64]),
                             reads=["ps%d" % bG], writes=["gend" + H])
                        Q.op("dve", lambda e: e.tensor_copy(out=gendl[h][:, 1:2], in_=ps[bG][:, 127:128]),
                             reads=["ps%d" % bG, "gend" + H], writes=["gend" + H])
                        Q.op("dve", lambda e: e.tensor_scalar(
                            out=wbt[h][:, 0:64], in0=ps[bG][:, 0:64], scalar1=mprev[:, h:h + 1], scalar2=80.0,
                            op0=ALU.subtract, op1=ALU.min), reads=["ps%d" % bG, "mprev" + H], writes=["wbt" + H])
                        Q.op("dve", lambda e: e.tensor_scalar(
                            out=wbt[h][:, 64:128], in0=ps[bG][:, 64:128], scalar1=gendl[h][:, 0:1], scalar2=80.0,
                            op0=ALU.subtract, op1=ALU.min), reads=["ps%d" % bG, "gend" + H, "wbt" + H], writes=["wbt" + H])
                        Q.op("act", lambda e: e.activation(out=argt[h][:], in_=argt[h][:], func=ACT.Exp, scale=-1.0),
                             reads=["argt" + H], writes=["argt" + H])
                        Q.op("act", lambda e: e.activation(out=wbt[h][:], in_=wbt[h][:], func=ACT.Exp, scale=-1.0),
                             reads=["wbt" + H], writes=["wbt" + H])
                        for c in range(2):
                            Q.op("act", lambda e, c=c: e.activation(
                                out=wcol[h][64 * c:64 * c + 64, 0:1], in_=gendl[h][64 * c:64 * c + 64, c:c + 1], func=ACT.Exp,
                                scale=-1.0, bias=cols[64 * c:64 * c + 64, h:h + 1]),
                                reads=["gend" + H, "cols", "wcol" + H], writes=["wcol" + H])
                        Q.op("dve", lambda e: e.tensor_copy(out=mprev[:, h:h + 1], in_=gendl[h][:, 1:2]),
                             reads=["gend" + H, "mprev" + H], writes=["mprev" + H])
                        Q.op("dve", lambda e: e.tensor_tensor(out=sctf[h][:], in0=ps[bS][:, 128:256], in1=argt[h][:], op=ALU.mult),
                             reads=["ps%d" % bS, "argt" + H], writes=["sctf" + H])
                        Q.op("dve", lambda e: e.tensor_tensor(out=sctb[h][:], in0=sctf[h][:], in1=maskbd[:], op=ALU.mult),
                             reads=["sctf" + H, "maskbd"], writes=["sctb" + H])
                        Q.op("dve", lambda e: e.tensor_scalar(out=kw[h][:], in0=ktm[:, h * 256:(h + 1) * 256],
                                                              scalar1=wcol[h][:, 0:1], scalar2=None, op0=ALU.mult),
                             reads=["ktm", "wcol" + H], writes=["kw" + H])
                        Q.op("dve", lambda e: e.tensor_tensor(
                            out=qst[h][:], in0=qTb[:, 2 * h:2 * h + 2, :], in1=wbt[h][:].unsqueeze(1).to_broadcast([128, 2, 128]),
                            op=ALU.mult), reads=["qTb", "wbt" + H], writes=["qst" + H])
                        bN = 2 + h % 2
                        Q.op("pe", lambda e: e.matmul(out=ps[bN][:, 0:257], lhsT=sctb[h][:], rhs=vaug[:, h, :],
                                                      start=True, stop=False),
                             reads=["sctb" + H, "vaug"], writes=["ps%d" % bN])
                        for c in range(2):
                            Q.begin()
                            for kc in range(2):
                                Q.op("pe", lambda e, c=c, kc=kc: e.matmul(
                                    out=ps[bN][64 * c:64 * c + 64, 0:257], lhsT=qst[h][:, kc, 64 * c:64 * c + 64],
                                    rhs=Cb[:, h, kc, :], start=False, stop=(c == 1 and kc == 1)),
                                    reads=["qst" + H, "Cb%d%d" % (h, kc)], writes=["ps%d" % bN])
                            Q.end()
                            for kc in range(2):
                                bU = bX
                                Q.op("pe", lambda e, bU=bU, c=c, kc=kc: e.matmul(
                                    out=ps[bU][:, 0:257], lhsT=kw[h][64 * c:64 * c + 64, kc * 128:(kc + 1) * 128],
                                    rhs=vaug[64 * c:64 * c + 64, h, :], start=True, stop=True),
                                    reads=["kw" + H, "vaug"], writes=["ps%d" % bU])
                                Q.op("dve", lambda e, bU=bU, c=c, kc=kc: e.scalar_tensor_tensor(
                                    out=Cf[:, h, kc, :], in0=Cf[:, h, kc, :], scalar=wbt[h][:, 64 * c + 63:64 * c + 64],
                                    in1=ps[bU][:, 0:257], op0=ALU.mult, op1=ALU.add),
                                    reads=["Cf%d%d" % (h, kc), "wbt" + H, "ps%d" % bU], writes=["Cf%d%d" % (h, kc)])
                                Q.op("act", lambda e, kc=kc: e.activation(out=Cb[:, h, kc, :], in_=Cf[:, h, kc, :], func=ACT.Copy),
                                     reads=["Cf%d%d" % (h, kc)], writes=["Cb%d%d" % (h, kc)])
                        Q.op("act", lambda e: e.activation(out=dn[h][:], in_=ps[bN][:, 256:257], func=ACT.Abs),
                             reads=["ps%d" % bN], writes=["dn" + H])
                        Q.op("dve", lambda e: e.tensor_tensor(out=dn[h][:], in0=dn[h][:], in1=emt[:, h:h + 1], op=ALU.max),
                             reads=["dn" + H, "emt"], writes=["dn" + H])
                        Q.op("dve", lambda e: e.reciprocal(out=dn[h][:], in_=dn[h][:]), reads=["dn" + H], writes=["dn" + H])
                        Q.op("act", lambda e: e.activation(out=hh[h][:], in_=ps[bN][:, 0:256], func=ACT.Copy, scale=dn[h][:, 0:1]),
                             reads=["ps%d" % bN, "dn" + H], writes=["hh" + H])
                        Q.op("act", lambda e: e.activation(out=junk3[h][:], in_=hh[h][:], func=ACT.Square, accum_out=ssqh[h][:]),
                             reads=["hh" + H], writes=["junk3" + H, "ssqh" + H])
                        Q.op("dve", lambda e: e.tensor_scalar(out=ssqh[h][:], in0=ssqh[h][:], scalar1=1.0 / 256, scalar2=EPS,
                                                              op0=ALU.mult, op1=ALU.add), reads=["ssqh" + H], writes=["ssqh" + H])
                        Q.op("act", lambda e: e.activation(out=ssqh[h][:], in_=ssqh[h][:], func=ACT.Sqrt),
                             reads=["ssqh" + H], writes=["ssqh" + H])
                        Q.op("dve", lambda e: e.reciprocal(out=ssqh[h][:], in_=ssqh[h][:]), reads=["ssqh" + H], writes=["ssqh" + H])
                        Q.op("dve", lambda e: e.scalar_tensor_tensor(
                            out=hn[h][:], in0=hh[h][:], scalar=ssqh[h][:, 0:1], in1=gml[:, h * 256:(h + 1) * 256],
                            op0=ALU.mult, op1=ALU.mult), reads=["hh" + H, "ssqh" + H, "gml"], writes=["hn" + H])
                        Q.op("dve", lambda e: e.tensor_tensor(out=ml[:, h * 256:(h + 1) * 256], in0=hn[h][:],
                                                              in1=so[:, h * 256:(h + 1) * 256], op=ALU.mult),
                             reads=["hn" + H, "so"], writes=["ml" + H])
                        return Q.steps

                    for pair in ((0, 1), (2, 3)):
                        chains = [head_chain(h) for h in pair]
                        while any(chains):
                            for ch_ in chains:
                                replay(ch_, 1)
                            replay(p0s, p0_per_round)
                            replay(p2s, p2_per_round)
                    b = lbank()
                    for blk in range(8):
                        S.op("pe", lambda e, b=b, blk=blk: e.transpose(
                            out=psb(b)[:, blk * 128:(blk + 1) * 128], in_=ml[:, blk * 128:(blk + 1) * 128], identity=identb[:]),
                            reads=["ml%d" % (blk // 2), "identb"], writes=["ps%d" % b])
                    S.op("act", lambda e, b=b: e.activation(out=mlT[:].rearrange("p b t -> p (b t)"), in_=psb(b)[:, :], func=ACT.Copy),
                         reads=["ps%d" % b], writes=["mlT"])
                    S.dma("pool", out=MlT_v[:, :, t0:t0 + 128], in_=mlT[:], reads=["mlT"])
            replay(p0s, 10 ** 9)
            replay(p2s, 10 ** 9)
            S.full_barrier()

    if 4 in phases:
        with ExitStack() as es4:
            def sb4(name, shape, dt):
                return es4.enter_context(nc.sbuf_tensor(uniq(name), list(shape), dt))
            wstgs = [sb4("wstg%d" % i, [128, D], F32) for i in range(2)]
            wpa = sb4("wpa", [128, 8, D], BF16)
            wpm = sb4("wpm", [128, 8, D], BF16)
            for (wsrc, wdst, wres) in ((w_proj_att, wpa, "wpa"), (w_proj_ml, wpm, "wpm")):
                wv = wsrc.rearrange("(k p) n -> p k n", p=128)
                for k in range(8):
                    wstg = wstgs[k % 2]
                    S.dma("sp", out=wstg[:], in_=wv[:, k, :], writes=["wstg%d" % (k % 2)])
                    if k % 2 == 0:
                        S.op("dve", lambda e, k=k, wdst=wdst, wstg=wstg: e.tensor_copy(out=wdst[:, k, :], in_=wstg[:]),
                             reads=["wstg0"], writes=[wres + "e"])
                    else:
                        S.op("act", lambda e, k=k, wdst=wdst, wstg=wstg: e.activation(out=wdst[:, k, :], in_=wstg[:], func=ACT.Copy),
                             reads=["wstg1"], writes=[wres + "o"])
            attg = sb4("attg", [128, 8, GT], BF16)
            mlg = sb4("mlg", [128, 8, GT], BF16)
            sg0s = [sb4("sg0_%d" % i, [128, GT], F32) for i in range(2)]
            sg1s = [sb4("sg1_%d" % i, [128, GT], F32) for i in range(2)]
            t1 = sb4("t1", [128, GT], F32)
            t2 = sb4("t2", [128, GT], F32)
            mrg = [sb4("mrg%d" % i, [128, GT], BF16) for i in range(2)]
            AttT_k = AttT.rearrange("(k p) t -> p k t", p=128)
            MlT_k = MlT.rearrange("(k p) t -> p k t", p=128)
            for g in range(NG):
                S.dma("sp", out=attg[:], in_=AttT_k[:, :, g * GT:(g + 1) * GT], writes=["attg"])
                S.dma("sp", out=mlg[:], in_=MlT_k[:, :, g * GT:(g + 1) * GT], writes=["mlg"])
                for nb in range(16):
                    bA = nbank()
                    for kc in range(8):
                        S.op("pe", lambda e, bA=bA, kc=kc, nb=nb: e.matmul(
                            out=ps[bA][:, 0:GT], lhsT=wpa[:, kc, nb * 128:(nb + 1) * 128], rhs=attg[:, kc, :],
                            start=(kc == 0), stop=(kc == 7)), reads=["wpae", "wpao", "attg"], writes=["ps%d" % bA], inc=(kc == 7))
                    bM = nbank()
                    for kc in range(8):
                        S.op("pe", lambda e, bM=bM, kc=kc, nb=nb: e.matmul(
                            out=ps[bM][:, 0:GT], lhsT=wpm[:, kc, nb * 128:(nb + 1) * 128], rhs=mlg[:, kc, :],
                            start=(kc == 0), stop=(kc == 7)), reads=["wpme", "wpmo", "mlg"], writes=["ps%d" % bM], inc=(kc == 7))
                    sg0, sg1 = sg0s[nb % 2], sg1s[nb % 2]
                    S.dma("sp", out=sg0[:], in_=Pg[nb * 128:(nb + 1) * 128, g * GT:(g + 1) * GT], writes=["sg0_%d" % (nb % 2)])
                    S.dma("sp", out=sg1[:], in_=Pg[2048 + nb * 128:2048 + (nb + 1) * 128, g * GT:(g + 1) * GT], writes=["sg1_%d" % (nb % 2)])
                    S.op("dve", lambda e, bA=bA, sg0=sg0: e.tensor_tensor(out=t1[:], in0=ps[bA][:, 0:GT], in1=sg0[:], op=ALU.mult),
                         reads=["ps%d" % bA, "sg0_%d" % (nb % 2)], writes=["t1"])
                    S.op("dve", lambda e, bM=bM, sg1=sg1: e.tensor_tensor(out=t2[:], in0=ps[bM][:, 0:GT], in1=sg1[:], op=ALU.mult),
                         reads=["ps%d" % bM, "sg1_%d" % (nb % 2)], writes=["t2"])
                    mi = nb % 2
                    S.op("dve", lambda e, mi=mi: e.tensor_tensor(out=mrg[mi][:], in0=t1[:], in1=t2[:], op=ALU.add),
                         reads=["t1", "t2"], writes=["mrg%d" % mi])
                    S.dma("pool", out=MgT[nb * 128:(nb + 1) * 128, g * GT:(g + 1) * GT], in_=mrg[mi][:], reads=["mrg%d" % mi])
            S.full_barrier()
        with ExitStack() as es4:
            def sb4(name, shape, dt):
                return es4.enter_context(nc.sbuf_tensor(uniq(name), list(shape), dt))
            wstgs = [sb4("wstg2_%d" % i, [128, D], F32) for i in range(2)]
            wo = sb4("wo", [128, KD, D], BF16)
            wv = w_out.rearrange("(k p) n -> p k n", p=128)
            for k in range(KD):
                wstg = wstgs[k % 2]
                S.dma("sp", out=wstg[:], in_=wv[:, k, :], writes=["wstg2_%d" % (k % 2)])
                if k % 2 == 0:
                    S.op("dve", lambda e, k=k, wstg=wstg: e.tensor_copy(out=wo[:, k, :], in_=wstg[:]), reads=["wstg2_0"], writes=["woe"])
                else:
                    S.op("act", lambda e, k=k, wstg=wstg: e.activation(out=wo[:, k, :], in_=wstg[:], func=ACT.Copy), reads=["wstg2_1"], writes=["woo"])
            mT = sb4("mT", [128, KD, GT], BF16)
            xt4 = sb4("xt4", [128, D], F32)
            x1t = sb4("x1t", [128, D], F32)
            MgT_k = MgT.rearrange("(k p) t -> p k t", p=128)
            for g in range(NG):
                S.dma("sp", out=mT[:], in_=MgT_k[:, :, g * GT:(g + 1) * GT], writes=["mT"])
                for ti in range(NTG):
                    t0 = g * GT + ti * 128
                    S.dma("sp", out=xt4[:], in_=x[t0:t0 + 128, :], writes=["xt4"])
                    for ncn in range(4):
                        bO = nbank()
                        for kc in range(KD):
                            S.op("pe", lambda e, bO=bO, kc=kc, ti=ti, ncn=ncn: e.matmul(
                                out=ps[bO][:, :], lhsT=mT[:, kc, ti * 128:(ti + 1) * 128], rhs=wo[:, kc, ncn * 512:(ncn + 1) * 512],
                                start=(kc == 0), stop=(kc == KD - 1)), reads=["mT", "woe", "woo"], writes=["ps%d" % bO], inc=(kc == KD - 1))
                        S.op("dve", lambda e, bO=bO, ncn=ncn: e.tensor_tensor(
                            out=x1t[:, ncn * 512:(ncn + 1) * 512], in0=ps[bO][:, :], in1=xt4[:, ncn * 512:(ncn + 1) * 512],
                            op=ALU.add), reads=["ps%d" % bO, "xt4"], writes=["x1t"])
                    S.dma("pool", out=X1[t0:t0 + 128, :], in_=x1t[:], reads=["x1t"])
            S.full_barrier()

    if 5 in phases:
        TGn = min(512, T)
        NS = TGn // 128
        NTG5 = T // TGn
        CG = 4
        NGR = 128 // CG
        TB = min(64, TGn)
        Gd2 = [dscr("Gd%d" % i, [NGR, 128, TGn, CG], BF16) for i in range(2)]
        with ExitStack() as es5:
            def sb5(name, shape, dt):
                return es5.enter_context(nc.sbuf_tensor(uniq(name), list(shape), dt))
            gffn = sb5("gffn", [128, D], F32)
            bload(gffn[:], norm_ffn_g[0:1, :], D, "gffn")
            keysT = sb5("keysT", [128, 16, 128], F32)
            iota_i = sb5("iota_i", [128, 128], I32)
            iota128 = sb5("iota128", [128, 128], F32)
            S.op("pool", lambda e: e.iota(out=iota_i[:], pattern=[[1, 128]], base=0, channel_multiplier=0), writes=["iota_i"])
            S.op("dve", lambda e: e.tensor_copy(out=iota128[:], in_=iota_i[:]), reads=["iota_i"], writes=["iota128"])
            iota128b = sb5("iota128b", [128, 128], BF16)
            S.op("dve", lambda e: e.tensor_copy(out=iota128b[:], in_=iota_i[:]), reads=["iota_i"], writes=["iota128b"])
            xn2Ts = [sb5("xn2T%d" % i, [128, KD, TGn], BF16) for i in range(2)]
            iTs = [sb5("iT%d" % i, [128, TGn], F32) for i in range(2)]
            jTs = [sb5("jT%d" % i, [128, TGn], F32) for i in range(2)]
            gTs = [sb5("gT%d" % i, [128, TGn], F32) for i in range(2)]
            ohj = [sb5("ohj%d" % i, [128, 128], BF16) for i in range(8)]
            ohi = [sb5("ohi%d" % i, [128, 128], BF16) for i in range(8)]
            gst = [sb5("gst%d" % i, [128, NGR, TB, CG], BF16) for i in range(2)]
            with nc.sbuf_tensor(uniq("kst"), [128, 16, 128], F32) as kst:
                S.dma("sp", out=kst[:], in_=peer_keys.rearrange("b n d -> n b d"), writes=["kst"])
                for blk in range(16):
                    b = nbank()
                    S.op("pe", lambda e, b=b, blk=blk: e.transpose(out=ps[b][:, 0:128], in_=kst[:, blk, :], identity=identf[:]),
                         reads=["kst", "identf"], writes=["ps%d" % b])
                    S.op("act", lambda e, b=b, blk=blk: e.activation(out=keysT[:, blk, :], in_=ps[b][:, 0:128], func=ACT.Copy),
                         reads=["ps%d" % b], writes=["keysT"])
                S.full_barrier()

            def routing_scope(tg):
                with ExitStack() as esr:
                    def sbr(name, shape, dt):
                        return esr.enter_context(nc.sbuf_tensor(uniq(name), list(shape), dt))
                    par = tg % 2
                    xn2T, iT, jT, gT = xn2Ts[par], iTs[par], jTs[par], gTs[par]
                    bufsets = []
                    for ci_ in range(2):
                        x1t = sbr("x1t5", [128, D], F32)
                        xn2 = sbr("xn2", [128, D], BF16)
                        ssq5 = sbr("ssq5", [128, 1], F32)
                        wqb = [sbr("wqb%d" % i, [128, KD, 128], BF16) for i in range(2)]
                        qpb = [sbr("qpb%d" % i, [128, 128], F32) for i in range(2)]
                        stop = sbr("stop", [128, 16, 16], F32)
                        itop = sbr("itop", [128, 16, 16], U32)
                        tmpb = sbr("tmpb", [128, 128], F32)
                        cand = sbr("cand", [128, 8, 256], F32)
                        tmpc2 = sbr("tmpc2", [128, 256], F32)
                        ctop = sbr("ctop", [128, 8, 16], F32)
                        cpos = sbr("cpos", [128, 8, 16], U32)
                        gz = sbr("gz", [128, 8], F32)
                        au = sbr("au", [128, 8, 16], U32)
                        bu = sbr("bu", [128, 8, 16], U32)
                        af_ = sbr("af_", [128, 8, 16], F32)
                        bf_ = sbr("bf_", [128, 8, 16], F32)
                        itf = sbr("itf", [128, 16, 16], F32)
                        eq = sbr("eq", [128, 8, 16, 16], F32)
                        iidx = sbr("iidx", [128, 8, 16], F32)
                        jidx = sbr("jidx", [128, 8, 16], F32)
                        stop4 = stop[:].rearrange("p (h c) r -> p h c r", c=2)
                        itf4 = itf[:].rearrange("p (h c) r -> p h c r", c=2)
                        bufsets.append(dict(x1t=x1t, xn2=xn2, ssq5=ssq5, wqb=wqb, qpb=qpb, stop=stop, itop=itop, tmpb=tmpb, cand=cand, tmpc2=tmpc2, ctop=ctop, cpos=cpos, gz=gz, au=au, bu=bu, af_=af_, bf_=bf_, itf=itf, eq=eq, iidx=iidx, jidx=jidx, stop4=stop4, itf4=itf4))
                    SHARED = ("identb", "identf", "gffn", "keysT", "iota128", "iota128b")

                    def tile_chain(s_, ci):
                        Q = Rec()
                        bs_ = bufsets[ci]
                        x1t = bs_["x1t"]
                        xn2 = bs_["xn2"]
                        ssq5 = bs_["ssq5"]
                        wqb = bs_["wqb"]
                        qpb = bs_["qpb"]
                        stop = bs_["stop"]
                        itop = bs_["itop"]
                        tmpb = bs_["tmpb"]
                        cand = bs_["cand"]
                        tmpc2 = bs_["tmpc2"]
                        ctop = bs_["ctop"]
                        cpos = bs_["cpos"]
                        gz = bs_["gz"]
                        au = bs_["au"]
                        bu = bs_["bu"]
                        af_ = bs_["af_"]
                        bf_ = bs_["bf_"]
                        itf = bs_["itf"]
                        eq = bs_["eq"]
                        iidx = bs_["iidx"]
                        jidx = bs_["jidx"]
                        stop4 = bs_["stop4"]
                        itf4 = bs_["itf4"]
                        cb_ = [0]

                        def cbank():
                            cb_[0] = (cb_[0] + 1) % 4
                            return 4 * ci + cb_[0]

                        def ren(n):
                            if n in SHARED or n.startswith("ps"):
                                return n
                            if n in ("xn2T", "iT", "jT", "gT"):
                                return "%s%d_s%d" % (n, par, s_)
                            return "%s_c%d" % (n, ci)

                        class QQ:
                            @staticmethod
                            def op(e, fn, reads=(), writes=(), inc=True):
                                Q.op(e, fn, reads=[ren(r) for r in reads], writes=[ren(w) for w in writes], inc=inc)

                            @staticmethod
                            def dma(q, out, in_, reads=(), writes=(), **kw):
                                Q.dma(q, out=out, in_=in_, reads=[ren(r) for r in reads], writes=[ren(w) for w in writes], **kw)

                        t0 = tg * TGn + s_ * 128
                        QQ.dma("sp", out=x1t[:], in_=X1[t0:t0 + 128, :], writes=["x1t5"])
                        QQ.op("act", lambda e: e.activation(out=xn2[:], in_=x1t[:], func=ACT.Square, accum_out=ssq5[:]),
                             reads=["x1t5"], writes=["xn2", "ssq5"])
                        QQ.op("dve", lambda e: e.tensor_scalar(out=ssq5[:], in0=ssq5[:], scalar1=1.0 / D, scalar2=EPS,
                                                              op0=ALU.mult, op1=ALU.add), reads=["ssq5"], writes=["ssq5"])
                        QQ.op("act", lambda e: e.activation(out=ssq5[:], in_=ssq5[:], func=ACT.Sqrt), reads=["ssq5"], writes=["ssq5"])
                        QQ.op("dve", lambda e: e.reciprocal(out=ssq5[:], in_=ssq5[:]), reads=["ssq5"], writes=["ssq5"])
                        QQ.op("dve", lambda e: e.scalar_tensor_tensor(out=xn2[:], in0=x1t[:], scalar=ssq5[:, 0:1], in1=gffn[:],
                                                                     op0=ALU.mult, op1=ALU.mult),
                             reads=["x1t5", "ssq5", "gffn"], writes=["xn2"])
                        for kq in range(4):
                            b = cbank()
                            Q.begin()
                            for j in range(4):
                                k = kq * 4 + j
                                QQ.op("pe", lambda e, k=k, j=j, b=b: e.transpose(out=psb(b)[:, j * 128:(j + 1) * 128],
                                                                                in_=xn2[:, k * 128:(k + 1) * 128],
                                                                                identity=identb[:]),
                                     reads=["xn2", "identb"], writes=["ps%d" % b])
                            Q.end()
                            QQ.op("act", lambda e, kq=kq, b=b, s_=s_: e.activation(
                                out=xn2T[:, kq * 4:(kq + 1) * 4, s_ * 128:(s_ + 1) * 128],
                                in_=psb(b)[:, 0:512].rearrange("p (j t) -> p j t", j=4), func=ACT.Copy),
                                reads=["ps%d" % b], writes=["xn2T"])
                        for blk in range(16):
                            sl = blk % 2
                            QQ.dma("sp", out=wqb[sl][:].rearrange("p k c -> p (k c)"), in_=WQs[blk].rearrange("p k c -> p (k c)"), writes=["wqb%d" % sl])
                            bq = cbank()
                            Q.begin()
                            for k in range(KD):
                                QQ.op("pe", lambda e, bq=bq, k=k, sl=sl, s_=s_: e.matmul(
                                    out=ps[bq][:, 0:128], lhsT=wqb[sl][:, k, :], rhs=xn2T[:, k, s_ * 128:(s_ + 1) * 128],
                                    start=(k == 0), stop=(k == KD - 1)), reads=["wqb%d" % sl, "xn2T"], writes=["ps%d" % bq], inc=(k == KD - 1))
                            Q.end()
                            QQ.op("act", lambda e, bq=bq, sl=sl: e.activation(out=qpb[sl][:], in_=ps[bq][:, 0:128], func=ACT.Copy),
                                 reads=["ps%d" % bq], writes=["qpb%d" % sl])
                            bs = cbank()
                            QQ.op("pe", lambda e, bs=bs, sl=sl, blk=blk: e.matmul(
                                out=ps[bs][:, 0:128], lhsT=qpb[sl][:], rhs=keysT[:, blk, :], start=True, stop=True),
                                reads=["qpb%d" % sl, "keysT"], writes=["ps%d" % bs])
                            pr = "ps%d" % bs
                            QQ.op("dve", lambda e, bs=bs, blk=blk: e.max(out=stop[:, blk, 0:8], in_=ps[bs][:, 0:128]),
                                 reads=[pr], writes=["stop"])
                            QQ.op("dve", lambda e, bs=bs, blk=blk: e.max_index(out=itop[:, blk, 0:8], in_max=stop[:, blk, 0:8],
                                                                              in_values=ps[bs][:, 0:128]),
                                 reads=[pr, "stop"], writes=["itop"])
                            QQ.op("dve", lambda e, bs=bs, blk=blk: e.match_replace(out=tmpb[:], in_to_replace=stop[:, blk, 0:8],
                                                                                  in_values=ps[bs][:, 0:128], imm_value=-1e30),
                                 reads=[pr, "stop"], writes=["tmpb"])
                            QQ.op("dve", lambda e, blk=blk: e.max(out=stop[:, blk, 8:16], in_=tmpb[:]), reads=["tmpb"], writes=["stop"])
                            QQ.op("dve", lambda e, blk=blk: e.max_index(out=itop[:, blk, 8:16], in_max=stop[:, blk, 8:16],
                                                                       in_values=tmpb[:]), reads=["tmpb", "stop"], writes=["itop"])
                        QQ.op("dve", lambda e: e.tensor_tensor(
                            out=cand[:].rearrange("p h (a b) -> p h a b", a=16),
                            in0=stop4[:, :, 0, :].unsqueeze(3).to_broadcast([128, 8, 16, 16]),
                            in1=stop4[:, :, 1, :].unsqueeze(2).to_broadcast([128, 8, 16, 16]), op=ALU.add),
                            reads=["stop"], writes=["cand"])
                        for h in range(8):
                            QQ.op("dve", lambda e, h=h: e.max(out=ctop[:, h, 0:8], in_=cand[:, h, :]), reads=["cand"], writes=["ctop"])
                            QQ.op("dve", lambda e, h=h: e.max_index(out=cpos[:, h, 0:8], in_max=ctop[:, h, 0:8], in_values=cand[:, h, :]),
                                 reads=["cand", "ctop"], writes=["cpos"])
                            QQ.op("dve", lambda e, h=h: e.match_replace(out=tmpc2[:], in_to_replace=ctop[:, h, 0:8],
                                                                       in_values=cand[:, h, :], imm_value=-1e30),
                                 reads=["cand", "ctop"], writes=["tmpc2"])
                            QQ.op("dve", lambda e, h=h: e.max(out=ctop[:, h, 8:16], in_=tmpc2[:]), reads=["tmpc2"], writes=["ctop"])
                            QQ.op("dve", lambda e, h=h: e.max_index(out=cpos[:, h, 8:16], in_max=ctop[:, h, 8:16], in_values=tmpc2[:]),
                                 reads=["tmpc2", "ctop"], writes=["cpos"])
                        QQ.op("dve", lambda e: e.tensor_scalar(out=au[:], in0=cpos[:], scalar1=4, scalar2=None,
                                                              op0=ALU.logical_shift_right), reads=["cpos"], writes=["au"])
                        QQ.op("dve", lambda e: e.tensor_scalar(out=bu[:], in0=cpos[:], scalar1=15, scalar2=None,
                                                              op0=ALU.bitwise_and), reads=["cpos"], writes=["bu"])
                        QQ.op("dve", lambda e: e.tensor_copy(out=af_[:], in_=au[:]), reads=["au"], writes=["af_"])
                        QQ.op("dve", lambda e: e.tensor_copy(out=bf_[:], in_=bu[:]), reads=["bu"], writes=["bf_"])
                        QQ.op("dve", lambda e: e.tensor_copy(out=itf[:], in_=itop[:]), reads=["itop"], writes=["itf"])
                        for (sel_f, c_, dst, dres) in ((af_, 0, iidx, "iidx"), (bf_, 1, jidx, "jidx")):
                            QQ.op("dve", lambda e, sel_f=sel_f: e.tensor_tensor(
                                out=eq[:], in0=sel_f[:].unsqueeze(3).to_broadcast([128, 8, 16, 16]),
                                in1=iota128[:, 0:16].unsqueeze(1).unsqueeze(1).to_broadcast([128, 8, 16, 16]), op=ALU.is_equal),
                                reads=["af_", "bf_", "iota128"], writes=["eq"])
                            QQ.op("dve", lambda e, c_=c_: e.tensor_tensor(
                                out=eq[:], in0=eq[:], in1=itf4[:, :, c_, :].unsqueeze(2).to_broadcast([128, 8, 16, 16]), op=ALU.mult),
                                reads=["eq", "itf"], writes=["eq"])
                            QQ.op("dve", lambda e, dst=dst: e.tensor_reduce(out=dst[:], in_=eq[:], axis=AX.X, op=ALU.add),
                                 reads=["eq"], writes=[dres])
                        QQ.op("dve", lambda e: e.tensor_tensor(out=ctop[:], in0=ctop[:], in1=ctop[:, :, 0:1].to_broadcast([128, 8, 16]),
                                                              op=ALU.subtract), reads=["ctop"], writes=["ctop"])
                        QQ.op("act", lambda e: e.activation(out=ctop[:], in_=ctop[:], func=ACT.Exp), reads=["ctop"], writes=["ctop"])
                        QQ.op("dve", lambda e: e.tensor_reduce(out=gz[:], in_=ctop[:], axis=AX.X, op=ALU.add), reads=["ctop"], writes=["gz"])
                        QQ.op("dve", lambda e: e.reciprocal(out=gz[:], in_=gz[:]), reads=["gz"], writes=["gz"])
                        QQ.op("dve", lambda e: e.tensor_tensor(out=ctop[:], in0=ctop[:], in1=gz[:].unsqueeze(2).to_broadcast([128, 8, 16]),
                                                              op=ALU.mult), reads=["ctop", "gz"], writes=["ctop"])
                        for (src, dstT, sres, dres) in ((iidx, iT, "iidx", "iT"), (jidx, jT, "jidx", "jT"), (ctop, gT, "ctop", "gT")):
                            b = cbank()
                            QQ.op("pe", lambda e, b=b, src=src: e.transpose(out=ps[b][:, 0:128], in_=src[:].rearrange("p h r -> p (h r)"),
                                                                           identity=identf[:]),
                                 reads=[sres, "identf"], writes=["ps%d" % b])
                            QQ.op("act", lambda e, b=b, dstT=dstT, s_=s_: e.activation(out=dstT[:, s_ * 128:(s_ + 1) * 128],
                                                                                    in_=ps[b][:, 0:128], func=ACT.Copy),
                                 reads=["ps%d" % b], writes=[dres])
                        return Q.steps

                    chains = [[], []]
                    for s_ in range(NS):
                        chains[s_ % 2].extend(tile_chain(s_, s_ % 2))
                    while any(chains):
                        for ch_ in chains:
                            replay(ch_, 1)
                    S.full_barrier()

            def gbuild_steps(tg):
                par = tg % 2
                iT, jT, gT = iTs[par], jTs[par], gTs[par]
                dsteps, psteps, asteps = [], [], []
                for t in range(TGn):
                    o = t % 8
                    Q = Rec()
                    Q.op("dve", lambda e, o=o, t=t: e.tensor_scalar(out=ohj[o][:], in0=iota128b[:], scalar1=jT[:, t:t + 1],
                                                                    scalar2=None, op0=ALU.is_equal),
                         reads=["iota128b", "jT%d_s%d" % (par, t // 128)], writes=["ohj%d" % o])
                    Q.op("dve", lambda e, o=o, t=t: e.tensor_scalar(out=ohi[o][:], in0=iota128b[:],
                                                                    scalar1=iT[:, t:t + 1], scalar2=gT[:, t:t + 1],
                                                                    op0=ALU.is_equal, op1=ALU.mult),
                         reads=["iota128b", "iT%d_s%d" % (par, t // 128), "gT%d_s%d" % (par, t // 128)], writes=["ohi%d" % o])
                    dsteps.append(Q.steps)
                    if t % 4 == 0:
                        bg = 6 + (t // 4) % 2
                    Q = Rec()
                    Q.op("pe", lambda e, bg=bg, o=o, t=t: e.matmul(out=ps[bg][:, (t % 4) * 128:(t % 4 + 1) * 128],
                                                                   lhsT=ohj[o][:], rhs=ohi[o][:], start=True, stop=True),
                         reads=["ohj%d" % o, "ohi%d" % o], writes=["ps%d" % bg])
                    psteps.append(Q.steps)
                    Q = Rec()
                    if t % 4 == 3:
                        sl = (t // TB) % 2
                        tq = (t % TB) // 4
                        Q.op("act", lambda e, bg=bg, sl=sl, tq=tq: e.activation(
                            out=gst[sl][:, :, tq * 4:(tq + 1) * 4, :].rearrange("p g t c -> p t g c"),
                            in_=ps[bg][:, :].rearrange("p (t g c) -> p t g c", t=4, g=NGR), func=ACT.Copy),
                            reads=["ps%d" % bg], writes=["gst%d" % sl])
                    if t % TB == TB - 1:
                        sl = (t // TB) % 2
                        tb0 = t - (TB - 1)
                        Q.dma("pool", out=Gd2[par][:, :, tb0:tb0 + TB, :].rearrange("g j t c -> j g (t c)"),
                              in_=gst[sl][:].rearrange("p g t c -> p g (t c)"), reads=["gst%d" % sl], writes=["Gd%d" % par])
                    asteps.append(Q.steps)
                LD, LA = 6, 4
                out = []
                for t in range(TGn + LD + LA):
                    if t < TGn:
                        out.extend(dsteps[t])
                    if 0 <= t - LD < TGn:
                        out.extend(psteps[t - LD])
                    if 0 <= t - LD - LA < TGn:
                        out.extend(asteps[t - LD - LA])
                return out

            def expert_scope(tg, nxt):
                pump_n = len(nxt) // ((NGR - 1) * (2 * CG + 2 * NS)) + 1
                par = tg % 2
                with ExitStack() as ese:
                    def sbe(name, shape, dt):
                        return ese.enter_context(nc.sbuf_tensor(uniq(name), list(shape), dt))
                    acc = sbe("acc", [128, NS, D], F32)
                    utc = [sbe("utc%d" % i, [128, KD, 128], BF16) for i in range(4)]
                    vg = [sbe("vg%d" % i, [128, CG, D], BF16) for i in range(2)]
                    gb = [sbe("gb%d" % i, [128, TGn, CG], BF16) for i in range(2)]
                    gl = [sbe("gl%d" % i, [128, TGn], F32) for i in range(2)]
                    ga = [sbe("ga%d" % i, [128, CG, TGn], BF16) for i in range(2)]
                    yt = sbe("yt", [128, D], F32)
                    pairs = ((0, 1), (2, 3))
                    pi = 0
                    for g in range(NGR):
                        gs = g % 2
                        S.dma("pool", out=gb[gs][:].rearrange("p t c -> p (t c)"), in_=Gd2[par][g].rearrange("j t c -> j (t c)"),
                              reads=["Gd%d" % par], writes=["gb%d" % gs])
                        for c in range(CG):
                            ich = g * CG + c
                            us = ich % 4
                            S.dma("sp", out=utc[us][:].rearrange("p k j -> p (k j)"), in_=UTs[ich], writes=["utc%d" % us])
                            S.dma("sp", out=vg[gs][:, c, :], in_=Vs[ich], writes=["vg%d_%d" % (gs, c)])
                            ba = 4 + ich % 2
                            for k in range(KD):
                                S.op("pe", lambda e, ba=ba, k=k, us=us: e.matmul(
                                    out=ps[ba][:, 0:TGn], lhsT=utc[us][:, k, :], rhs=xn2Ts[par][:, k, :],
                                    start=(k == 0), stop=(k == KD - 1)), reads=["utc%d" % us] + ["xn2T%d_s%d" % (par, q_) for q_ in range(NS)], writes=["ps%d" % ba], inc=(k == KD - 1))
                            replay(nxt, pump_n)
                            gi = ich % 2
                            S.op("act", lambda e, ba=ba, gi=gi: e.activation(out=gl[gi][:], in_=ps[ba][:, 0:TGn], func=ACT.Gelu),
                                 reads=["ps%d" % ba], writes=["gl%d" % gi])
                            S.op("dve", lambda e, gi=gi, gs=gs, c=c: e.tensor_tensor(
                                out=ga[gs][:, c, :], in0=gl[gi][:], in1=gb[gs][:, :, c], op=ALU.mult),
                                reads=["gl%d" % gi, "gb%d" % gs], writes=["ga%d_%d" % (gs, c)])
                            replay(nxt, pump_n)
                        for s_ in range(NS):
                            for hf in range(2):
                                pr = pairs[pi]
                                pi ^= 1
                                for c in range(CG):
                                    for n2 in range(2):
                                        c0 = hf * 1024 + n2 * 512
                                        S.op("pe", lambda e, pr=pr, n2=n2, gs=gs, c=c, s_=s_, c0=c0: e.matmul(
                                            out=ps[pr[n2]][:, :], lhsT=ga[gs][:, c, s_ * 128:(s_ + 1) * 128],
                                            rhs=vg[gs][:, c, c0:c0 + 512], start=(c == 0), stop=(c == CG - 1)),
                                            reads=["ga%d_%d" % (gs, c), "vg%d_%d" % (gs, c)], writes=["ps%d" % pr[n2]], inc=(c == CG - 1 and n2 == 1))
                                replay(nxt, pump_n)
                                for n2 in range(2):
                                    c0 = hf * 1024 + n2 * 512
                                    ares = "acc%d_%d" % (s_, hf * 2 + n2)
                                    if g == 0:
                                        S.op("act", lambda e, pr=pr, n2=n2, s_=s_, c0=c0: e.activation(
                                            out=acc[:, s_, c0:c0 + 512], in_=ps[pr[n2]][:, :], func=ACT.Copy),
                                            reads=["ps%d" % pr[n2]], writes=[ares])
                                    else:
                                        S.op("dve", lambda e, pr=pr, n2=n2, s_=s_, c0=c0: e.tensor_tensor(
                                            out=acc[:, s_, c0:c0 + 512], in0=ps[pr[n2]][:, :], in1=acc[:, s_, c0:c0 + 512], op=ALU.add),
                                            reads=["ps%d" % pr[n2], ares], writes=[ares])
                    for s_ in range(NS):
                        t0 = tg * TGn + s_ * 128
                        S.dma("sp", out=yt[:], in_=X1[t0:t0 + 128, :], writes=["yt"])
                        S.op("dve", lambda e, s_=s_: e.tensor_tensor(out=yt[:], in0=yt[:], in1=acc[:, s_, :], op=ALU.add),
                             reads=["yt"] + ["acc%d_%d" % (s_, q) for q in range(4)], writes=["yt"])
                        S.dma("pool", out=y[t0:t0 + 128, :], in_=yt[:], reads=["yt"])
                    replay(nxt, 10 ** 9)
                    S.full_barrier()

            routing_scope(0)
            replay(gbuild_steps(0), 10 ** 9)
            S.full_barrier()
            for tg in range(NTG5):
                if tg + 1 < NTG5:
                    routing_scope(tg + 1)
                    nxt = gbuild_steps(tg + 1)
                else:
                    nxt = []
                expert_scope(tg, nxt)
    S.barrier("sp")
    es.close()
    return nc


_NC_CACHE = {}


def kernel(**inputs):
    NSEQ, SEQ, NCORES = 2, 2048, 8
    if "nc" not in _NC_CACHE:
        _NC_CACHE["nc"] = build_program(NSEQ, SEQ)
    nc = _NC_CACHE["nc"]
    f = lambda a: np.ascontiguousarray(np.asarray(a, dtype=np.float32))
    shared = {}
    for k in ("norm_mix_g", "b_in", "conv_b", "q_norm_g", "k_norm_g", "sinks", "ml_norm_g", "norm_ffn_g"):
        shared[k] = f(inputs[k]).reshape(1, -1)
    for k in ("w_in", "conv_w", "w_proj_att", "w_proj_ml", "w_out", "w_peer_q", "peer_u", "peer_v"):
        shared[k] = f(inputs[k][0])
    shared["peer_keys"] = f(inputs["peer_keys"]).reshape(16, 128, 128)
    xs = f(inputs["x"]).reshape(NCORES, NSEQ * SEQ, D)
    in_maps = [dict(shared, x=xs[c]) for c in range(NCORES)]
    res = run_bass_kernel_spmd(nc, in_maps, core_ids=list(range(NCORES)))
    out = np.stack([np.asarray(r["y"]) for r in res.results], axis=0)
    return out.reshape(NCORES * NSEQ, SEQ, D).astype(np.float32)
```
